# Optimizing a Trainium2 kernel written in Bass

```python
import jax, jax.numpy as jnp
from jax import lax
import numpy as np

D_MODEL = 2048
BATCH = 1
SEQ = 16384
DEPTH = 2

BRANCH_W = D_MODEL // 2
N_BRANCH = 3
EPS = 1e-6
CONV_W = 4
LRU_BLOCKS = 16
LRU_BLOCK = BRANCH_W // LRU_BLOCKS
LRU_C = 8.0
GDN_HEADS = 8
GDN_HEAD_DIM = BRANCH_W // GDN_HEADS
GDN_CHUNK = 64
RWKV_HEAD = 64
RWKV_HEADS = BRANCH_W // RWKV_HEAD
RWKV_W_RANK = 64
RWKV_A_RANK = 64
RWKV_G_RANK = 160
RWKV_GN_EPS = 64e-5
RWKV_COLS = 3 * BRANCH_W + RWKV_W_RANK + RWKV_A_RANK + RWKV_G_RANK
D_FF = ((8 * D_MODEL // 3 + 255) // 256) * 256
IN_SIZES = (BRANCH_W, BRANCH_W,
            3 * BRANCH_W, BRANCH_W, GDN_HEADS, GDN_HEADS,
            RWKV_COLS,
            N_BRANCH * D_MODEL)
N_IN = sum(IN_SIZES)

kernel_name = 'hybrid_rglru_gdn_rwkv7_block'


def split_cols(t, sizes):
    idx = [int(i) for i in np.cumsum(sizes)[:-1]]
    return jnp.split(t, idx, axis=-1)


def rms_norm(x, w):
    xf = x.astype(jnp.float32)
    y = xf * lax.rsqrt(jnp.mean(xf * xf, axis=-1, keepdims=True) + EPS)
    return (y * w.astype(jnp.float32)).astype(x.dtype)


def l2_normalize(t):
    return t * lax.rsqrt(jnp.sum(t * t, axis=-1, keepdims=True) + EPS)


def causal_depthwise_conv(x, w, b=None):
    k = w.shape[0]
    xp = jnp.pad(x, ((0, 0), (k - 1, 0), (0, 0)))
    y = lax.conv_general_dilated(xp, w[:, None, :].astype(x.dtype), window_strides=(1,), padding='VALID',
                                 dimension_numbers=('NWC', 'WIO', 'NWC'), feature_group_count=x.shape[-1])
    return y if b is None else y + b.astype(x.dtype)


def rglru_branch(x_in, gate_in, conv_w, conv_b, w_a, b_a, w_x, b_x, lam):
    dtype = x_in.dtype
    xc = causal_depthwise_conv(x_in, conv_w, conv_b).astype(jnp.float32)
    B, S, C = xc.shape
    xb = xc.reshape(B, S, LRU_BLOCKS, LRU_BLOCK)
    r = jax.nn.sigmoid(jnp.einsum('bsgi,gij->bsgj', xb, w_a.astype(jnp.float32)).reshape(B, S, C) + b_a)
    i = jax.nn.sigmoid(jnp.einsum('bsgi,gij->bsgj', xb, w_x.astype(jnp.float32)).reshape(B, S, C) + b_x)
    log_a = -LRU_C * r * jax.nn.softplus(-lam.astype(jnp.float32))
    a = jnp.exp(log_a)
    u = jnp.sqrt(-jnp.expm1(2.0 * log_a)) * (i * xc)

    def combine(left, right):
        a_l, b_l = left
        a_r, b_r = right
        return a_l * a_r, a_r * b_l + b_r

    _, h = lax.associative_scan(combine, (a, u), axis=1)
    return (jax.nn.gelu(gate_in.astype(jnp.float32)) * h).astype(dtype)


def chunk_gated_delta_rule(q, k, v, g, beta):
    B, S, H, Dk = q.shape
    Dv = v.shape[-1]
    C = GDN_CHUNK
    N = S // C

    def to_chunks(t):
        return jnp.moveaxis(t.reshape(B, N, C, H, -1), 3, 1)

    q, k, v = to_chunks(q), to_chunks(k), to_chunks(v)
    g = to_chunks(g[..., None])[..., 0]
    beta = to_chunks(beta[..., None])[..., 0]
    gc = jnp.cumsum(g, axis=-1)
    causal = jnp.tril(jnp.ones((C, C), bool))
    strict = jnp.tril(jnp.ones((C, C), bool), -1)
    decay = jnp.exp(jnp.where(causal, gc[..., :, None] - gc[..., None, :], -jnp.inf))
    kk = jnp.einsum('bhnid,bhnjd->bhnij', k, k)
    m = jnp.where(strict, beta[..., :, None] * kk * decay, 0.0)
    a_mat = m + jnp.eye(C, dtype=m.dtype)
    rhs = jnp.concatenate([beta[..., None] * v, (beta * jnp.exp(gc))[..., None] * k], axis=-1)
    sol = lax.linalg.triangular_solve(a_mat, rhs, left_side=True, lower=True, unit_diagonal=True)
    u_v, w_k = sol[..., :Dv], sol[..., Dv:]
    attn = jnp.where(causal, jnp.einsum('bhnid,bhnjd->bhnij', q, k) * decay, 0.0)
    q_dec = q * jnp.exp(gc)[..., None]
    k_dec = k * jnp.exp(gc[..., -1:] - gc)[..., None]
    g_last = jnp.exp(gc[..., -1])
    xs = tuple(jnp.moveaxis(t, 2, 0) for t in (u_v, w_k, attn, q_dec, k_dec, g_last))

    def step(state, inp):
        u_v_c, w_k_c, attn_c, q_c, k_c, gl = inp
        u = u_v_c - jnp.einsum('bhck,bhkv->bhcv', w_k_c, state)
        o = jnp.einsum('bhck,bhkv->bhcv', q_c, state) + jnp.einsum('bhij,bhjv->bhiv', attn_c, u)
        state = state * gl[..., None, None] + jnp.einsum('bhck,bhcv->bhkv', k_c, u)
        return state, o

    s0 = jnp.zeros((B, H, Dk, Dv), q.dtype)
    _, o = lax.scan(step, s0, xs)
    return jnp.transpose(o, (1, 0, 3, 2, 4)).reshape(B, S, H, Dv)


def gated_deltanet_branch(qkv, z, beta_raw, alpha_raw, conv_w, a_log, dt_bias, norm_w):
    dtype = qkv.dtype
    B, S, _ = qkv.shape
    qkv = jax.nn.silu(causal_depthwise_conv(qkv, conv_w).astype(jnp.float32))
    q, k, v = (t.reshape(B, S, GDN_HEADS, GDN_HEAD_DIM) for t in jnp.split(qkv, 3, axis=-1))
    q = l2_normalize(q) * (GDN_HEAD_DIM ** -0.5)
    k = l2_normalize(k)
    beta = jax.nn.sigmoid(beta_raw.astype(jnp.float32))
    g = -jnp.exp(a_log.astype(jnp.float32)) * jax.nn.softplus(alpha_raw.astype(jnp.float32) + dt_bias.astype(jnp.float32))
    o = chunk_gated_delta_rule(q, k, v, g, beta)
    o = o * lax.rsqrt(jnp.mean(o * o, axis=-1, keepdims=True) + EPS) * norm_w.astype(jnp.float32)
    o = o * jax.nn.silu(z.astype(jnp.float32)).reshape(B, S, GDN_HEADS, GDN_HEAD_DIM)
    return o.reshape(B, S, BRANCH_W).astype(dtype)


def rwkv7_branch(slab, mu, w0, w_up, a0, a_up, g_up, k_k, k_a, r_k, ln_w, ln_b):
    dtype = slab.dtype
    f32 = jnp.float32
    s = slab.astype(f32)
    prev = jnp.pad(s, ((0, 0), (1, 0), (0, 0)))[:, :-1]
    s = s + (prev - s) * mu.astype(f32)
    r, k, v, w_lo, a_lo, g_lo = split_cols(s, (BRANCH_W, BRANCH_W, BRANCH_W, RWKV_W_RANK, RWKV_A_RANK, RWKV_G_RANK))
    B, S, _ = r.shape
    w = -jax.nn.softplus(-(w0.astype(f32) + jnp.tanh(w_lo) @ w_up.astype(f32))) - 0.5
    a = jax.nn.sigmoid(a0.astype(f32) + a_lo @ a_up.astype(f32))
    g = jax.nn.sigmoid(g_lo) @ g_up.astype(f32)

    def heads(t):
        return t.reshape(B, S, RWKV_HEADS, RWKV_HEAD)

    kk = l2_normalize(heads(k * k_k.astype(f32)))
    k = k * (1.0 + (a - 1.0) * k_a.astype(f32))
    r_h, k_h, v_h, a_h = heads(r), heads(k), heads(v), heads(a)
    dec = heads(jnp.exp(-jnp.exp(w)))
    xs = tuple(jnp.moveaxis(t, 1, 0) for t in (r_h, dec, k_h, v_h, kk, a_h))

    def step(state, inp):
        r_t, d_t, k_t, v_t, kk_t, a_t = inp
        sa = jnp.einsum('bhvk,bhk->bhv', state, -kk_t)
        state = (state * d_t[:, :, None, :] + sa[..., None] * (kk_t * a_t)[:, :, None, :]
                 + v_t[..., None] * k_t[:, :, None, :])
        return state, jnp.einsum('bhvk,bhk->bhv', state, r_t)

    s0 = jnp.zeros((B, RWKV_HEADS, RWKV_HEAD, RWKV_HEAD), f32)
    _, out = lax.scan(step, s0, xs)
    out = jnp.moveaxis(out, 0, 1)
    mean = jnp.mean(out, axis=-1, keepdims=True)
    var = jnp.mean(jnp.square(out - mean), axis=-1, keepdims=True)
    out = ((out - mean) * lax.rsqrt(var + RWKV_GN_EPS)).reshape(B, S, BRANCH_W) * ln_w.astype(f32) + ln_b.astype(f32)
    bonus = jnp.sum(r_h * k_h * r_k.astype(f32), axis=-1, keepdims=True) * v_h
    return ((out + bonus.reshape(B, S, BRANCH_W)) * g).astype(dtype)


def hybrid_mixer(h, w_in, lru_conv_w, lru_conv_b, lru_w_a, lru_b_a, lru_w_x, lru_b_x, lru_lambda,
                 gdn_conv_w, gdn_a_log, gdn_dt_bias, gdn_norm_w,
                 rwkv_mu, rwkv_w0, rwkv_w_up, rwkv_a0, rwkv_a_up, rwkv_g_up, rwkv_k_k, rwkv_k_a, rwkv_r_k,
                 rwkv_ln_w, rwkv_ln_b, w_branch, w_out):
    B, S, D = h.shape
    p = h @ w_in
    lru_x, lru_g, qkv, z, beta_raw, alpha_raw, rw, gate_raw = split_cols(p, IN_SIZES)
    y_a = rglru_branch(lru_x, lru_g, lru_conv_w, lru_conv_b, lru_w_a, lru_b_a, lru_w_x, lru_b_x, lru_lambda)
    y_b = gated_deltanet_branch(qkv, z, beta_raw, alpha_raw, gdn_conv_w, gdn_a_log, gdn_dt_bias, gdn_norm_w)
    y_c = rwkv7_branch(rw, rwkv_mu, rwkv_w0, rwkv_w_up, rwkv_a0, rwkv_a_up, rwkv_g_up, rwkv_k_k, rwkv_k_a,
                       rwkv_r_k, rwkv_ln_w, rwkv_ln_b)
    gates = jax.nn.sigmoid(gate_raw.astype(jnp.float32)).astype(h.dtype).reshape(B, S, N_BRANCH, D)
    mixed = (gates[:, :, 0] * (y_a @ w_branch[0]) + gates[:, :, 1] * (y_b @ w_branch[1])
             + gates[:, :, 2] * (y_c @ w_branch[2]))
    return mixed @ w_out


def swiglu(h, w_gate, w_up, w_down):
    return (jax.nn.silu(h @ w_gate) * (h @ w_up)) @ w_down


def setup_inputs(seed: int = 0) -> dict:
    key = jax.random.key(seed)
    ks = iter(jax.random.split(key, 48))
    f32 = jnp.float32
    L, D, BW = DEPTH, D_MODEL, BRANCH_W

    def normal(shape, scale):
        return jax.random.normal(next(ks), shape, f32) * scale

    def unif(shape, lo, hi):
        return jax.random.uniform(next(ks), shape, f32, lo, hi)

    def gain(shape):
        return 1.0 + normal(shape, 0.02)

    u = unif((L, BW), 0.9, 0.999)
    a_base = u ** (1.0 / LRU_C)
    lru_lambda = jnp.log(a_base) - jnp.log1p(-a_base)
    dt = jnp.exp(unif((L, GDN_HEADS), float(np.log(1e-3)), float(np.log(1e-1))))
    gdn_dt_bias = dt + jnp.log(-jnp.expm1(-dt))
    return {
        'x': jax.random.normal(next(ks), (BATCH, SEQ, D), f32),
        'norm_mix_pre': gain((L, D)),
        'norm_mix_post': gain((L, D)),
        'norm_ffn_pre': gain((L, D)),
        'norm_ffn_post': gain((L, D)),
        'w_in': normal((L, D, N_IN), D ** -0.5),
        'lru_conv_w': normal((L, CONV_W, BW), CONV_W ** -0.5),
        'lru_conv_b': normal((L, BW), 0.02),
        'lru_w_a': normal((L, LRU_BLOCKS, LRU_BLOCK, LRU_BLOCK), LRU_BLOCK ** -0.5),
        'lru_b_a': normal((L, BW), 0.02),
        'lru_w_x': normal((L, LRU_BLOCKS, LRU_BLOCK, LRU_BLOCK), LRU_BLOCK ** -0.5),
        'lru_b_x': normal((L, BW), 0.02),
        'lru_lambda': lru_lambda,
        'gdn_conv_w': normal((L, CONV_W, 3 * BW), CONV_W ** -0.5),
        'gdn_a_log': jnp.log(unif((L, GDN_HEADS), 1.0, 16.0)),
        'gdn_dt_bias': gdn_dt_bias,
        'gdn_norm_w': gain((L, GDN_HEAD_DIM)),
        'rwkv_mu': unif((L, RWKV_COLS), 0.0, 1.0),
        'rwkv_w0': unif((L, BW), -6.0, -1.0),
        'rwkv_w_up': normal((L, RWKV_W_RANK, BW), 0.05),
        'rwkv_a0': normal((L, BW), 0.1),
        'rwkv_a_up': normal((L, RWKV_A_RANK, BW), 0.05),
        'rwkv_g_up': normal((L, RWKV_G_RANK, BW), RWKV_G_RANK ** -0.5),
        'rwkv_k_k': 0.85 + normal((L, BW), 0.02),
        'rwkv_k_a': gain((L, BW)),
        'rwkv_r_k': normal((L, RWKV_HEADS, RWKV_HEAD), 0.1),
        'rwkv_ln_w': gain((L, BW)),
        'rwkv_ln_b': normal((L, BW), 0.02),
        'w_branch': normal((L, N_BRANCH, BW, D), BW ** -0.5),
        'w_out': normal((L, D, D), D ** -0.5),
        'ffn_w_gate': normal((L, D, D_FF), D ** -0.5),
        'ffn_w_up': normal((L, D, D_FF), D ** -0.5),
        'ffn_w_down': normal((L, D_FF, D), D_FF ** -0.5),
    }


def reference(x, norm_mix_pre, norm_mix_post, norm_ffn_pre, norm_ffn_post, w_in,
              lru_conv_w, lru_conv_b, lru_w_a, lru_b_a, lru_w_x, lru_b_x, lru_lambda,
              gdn_conv_w, gdn_a_log, gdn_dt_bias, gdn_norm_w,
              rwkv_mu, rwkv_w0, rwkv_w_up, rwkv_a0, rwkv_a_up, rwkv_g_up, rwkv_k_k, rwkv_k_a, rwkv_r_k,
              rwkv_ln_w, rwkv_ln_b, w_branch, w_out, ffn_w_gate, ffn_w_up, ffn_w_down):
    for l in range(DEPTH):
        h = rms_norm(x, norm_mix_pre[l])
        h = hybrid_mixer(h, w_in[l], lru_conv_w[l], lru_conv_b[l], lru_w_a[l], lru_b_a[l], lru_w_x[l], lru_b_x[l],
                         lru_lambda[l], gdn_conv_w[l], gdn_a_log[l], gdn_dt_bias[l], gdn_norm_w[l],
                         rwkv_mu[l], rwkv_w0[l], rwkv_w_up[l], rwkv_a0[l], rwkv_a_up[l], rwkv_g_up[l], rwkv_k_k[l],
                         rwkv_k_a[l], rwkv_r_k[l], rwkv_ln_w[l], rwkv_ln_b[l], w_branch[l], w_out[l])
        x = x + rms_norm(h, norm_mix_post[l])
        h = rms_norm(x, norm_ffn_pre[l])
        h = swiglu(h, ffn_w_gate[l], ffn_w_up[l], ffn_w_down[l])
        x = x + rms_norm(h, norm_ffn_post[l])
    return x
```

```python
import ml_dtypes
import numpy as np
from contextlib import ExitStack
import concourse.bass as bass
import concourse.mybir as mybir
from concourse.bass_utils import run_bass_kernel_spmd

F32 = mybir.dt.float32
BF16 = mybir.dt.bfloat16
ALU = mybir.AluOpType
AF = mybir.ActivationFunctionType

ENGS = ("pe", "act", "dve", "pool", "sp")


class _Op:
    __slots__ = ("eng", "fn", "deps", "dma", "chan", "cnt", "inc", "idx")

    def __init__(self, eng, fn, dma, chan):
        self.eng = eng
        self.fn = fn
        self.deps = []
        self.dma = dma
        self.chan = chan
        self.cnt = 0
        self.inc = False
        self.idx = 0


class G:
    def __init__(self, nc):
        self.nc = nc
        self.ops = {e: [] for e in ENGS}
        self.lastw = {}
        self.rds = {}
        self.chan_cnt = {}
        self.stack = ExitStack()
        self.n_sb = 0

    def sb(self, shape, dt=F32, name=None):
        self.n_sb += 1
        return self.stack.enter_context(self.nc.sbuf_tensor(name or f"sb{self.n_sb}", list(shape), dt))

    def ps(self, shape, dt=F32, name=None):
        self.n_sb += 1
        return self.stack.enter_context(self.nc.psum_tensor(name or f"ps{self.n_sb}", list(shape), dt))

    def op(self, eng, fn, reads=(), writes=(), dma=False, chan=None):
        o = _Op(eng, fn, dma, chan)
        deps = set()
        for k in reads:
            w = self.lastw.get(k)
            if w is not None:
                deps.add(w)
        for k in writes:
            w = self.lastw.get(k)
            if w is not None:
                deps.add(w)
            for r in self.rds.get(k, ()):
                deps.add(r)
        o.idx = len(self.ops[eng])
        self.ops[eng].append(o)
        me = (eng, o.idx)
        deps.discard(me)
        o.deps = list(deps)
        if dma:
            assert chan is not None
            c = self.chan_cnt.get(chan, 0) + 1
            self.chan_cnt[chan] = c
            o.cnt = c
        for k in reads:
            self.rds.setdefault(k, []).append(me)
        for k in writes:
            self.lastw[k] = me
            self.rds[k] = []
        return o

    def dma(self, eng, out, in_, reads=(), writes=(), chan=None, **kw):
        if chan is None:
            chan = ("ch",) + tuple(writes) + tuple(reads)
        return self.op(eng, lambda e: e.dma_start(out=out, in_=in_, **kw), reads, writes, dma=True, chan=chan)

    def emit(self, final_waits=True):
        nc = self.nc
        ops = self.ops
        for e in ENGS:
            for o in ops[e]:
                for (de, di) in o.deps:
                    d = ops[de][di]
                    if d.dma:
                        continue
                    if de == "pe" and e == "pe":
                        continue
                    d.inc = True
        for e in ENGS:
            c = 0
            for o in ops[e]:
                if o.dma:
                    continue
                if o.inc:
                    c += 1
                    o.cnt = c
        st = self.stack
        esem = {e: st.enter_context(nc.semaphore(f"s_{e}")) for e in ENGS}
        csem = {}
        for ch in self.chan_cnt:
            csem[ch] = st.enter_context(nc.semaphore(f"c{len(csem)}"))
        engobj = {"pe": "tensor", "act": "scalar", "dve": "vector", "pool": "gpsimd", "sp": "sync"}
        self.n_waits = 0
        with nc.Block() as block:
            def run(ename):
                def body(eng):
                    waited = {}
                    for o in ops[ename]:
                        need = {}
                        for (de, di) in o.deps:
                            d = ops[de][di]
                            if d.dma:
                                key = ("c", d.chan)
                                val = 16 * d.cnt
                                sem = csem[d.chan]
                            else:
                                if de == "pe" and ename == "pe":
                                    continue
                                key = ("e", de)
                                val = d.cnt
                                sem = esem[de]
                            if val > need.get(key, (0, None))[0]:
                                need[key] = (val, sem)
                        for key, (val, sem) in need.items():
                            if waited.get(key, 0) >= val:
                                continue
                            waited[key] = val
                            eng.wait_ge(sem, val)
                            self.n_waits += 1
                        ins = o.fn(eng)
                        if o.dma:
                            ins.then_inc(csem[o.chan], 16)
                        elif o.inc:
                            ins.then_inc(esem[ename], 1)
                    if final_waits and ename == "sp":
                        for ch, c in self.chan_cnt.items():
                            eng.wait_ge(csem[ch], 16 * c)
                return body
            for e in ENGS:
                getattr(block, engobj[e])(run(e))
        self.stack.close()


T = 256
NCH = 4
C = 64
D = 2048
NCOL = 1442
NPV = 36
NM = 10
NCM = 6
EPS = 1e-6

GROUPS = [("lx", 0, 128, 3), ("lg", 128, 128, 0), ("q", 256, 128, 3), ("k", 384, 128, 3), ("v", 512, 128, 3),
          ("z", 640, 128, 0), ("r", 768, 128, 1), ("rk", 896, 128, 1), ("rv", 1024, 128, 1), ("wa", 1152, 128, 1),
          ("g1", 1280, 128, 1), ("g2", 1408, 32, 1), ("ba", 1440, 2, 0)]


class Bld:
    def __init__(self, g):
        self.g = g

    @staticmethod
    def k(ap):
        return ap.tensor.name

    def mm(self, out, lhsT, rhs, start=True, stop=True):
        self.g.op("pe", lambda e: e.matmul(out, lhsT, rhs, start=start, stop=stop),
                  reads=[self.k(lhsT), self.k(rhs)], writes=[self.k(out)])

    def tr(self, out, in_, ident):
        self.g.op("pe", lambda e: e.transpose(out, in_, ident), reads=[self.k(in_), self.k(ident)], writes=[self.k(out)])

    def act(self, out, in_, func, bias=None, scale=None, eng="act"):
        kw = {}
        rd = [self.k(in_)]
        if bias is not None:
            kw["bias"] = bias
            if not isinstance(bias, float):
                rd.append(self.k(bias))
        if scale is not None:
            kw["scale"] = scale
            if not isinstance(scale, float):
                rd.append(self.k(scale))
        self.g.op("act", lambda e: e.activation(out, in_, func, **kw), reads=rd, writes=[self.k(out)])

    def ts(self, out, in0, s1, s2, op0, op1=None, eng="dve"):
        rd = [self.k(in0)]
        for s in (s1, s2):
            if s is not None and not isinstance(s, float):
                rd.append(self.k(s))
        if op1 is None:
            fn = lambda e: e.tensor_scalar(out, in0, s1, None, op0)
        else:
            fn = lambda e: e.tensor_scalar(out, in0, s1, s2, op0, op1)
        self.g.op(eng, fn, reads=rd, writes=[self.k(out)])

    def tt(self, out, a, b, op, eng="dve"):
        self.g.op(eng, lambda e: e.tensor_tensor(out, a, b, op), reads=[self.k(a), self.k(b)], writes=[self.k(out)])

    def stt(self, out, in0, sc, in1, op0, op1):
        rd = [self.k(in0), self.k(in1)]
        if not isinstance(sc, float):
            rd.append(self.k(sc))
        self.g.op("dve", lambda e: e.scalar_tensor_tensor(out, in0, sc, in1, op0, op1), reads=rd, writes=[self.k(out)])

    def scan(self, out, d0, d1, init):
        rd = [self.k(d0), self.k(d1)]
        if not isinstance(init, float):
            rd.append(self.k(init))
        self.g.op("dve", lambda e: e.tensor_tensor_scan(out, d0, d1, init, ALU.mult, ALU.add), reads=rd, writes=[self.k(out)])

    def cp(self, out, in_, eng="dve"):
        if eng == "act":
            self.g.op("act", lambda e: e.copy(out, in_), reads=[self.k(in_)], writes=[self.k(out)])
        else:
            self.g.op(eng, lambda e: e.tensor_copy(out, in_), reads=[self.k(in_)], writes=[self.k(out)])

    def recip(self, out, in_):
        self.g.op("dve", lambda e: e.reciprocal(out, in_), reads=[self.k(in_)], writes=[self.k(out)])

    def memset(self, ap, v, eng="pool"):
        self.g.op(eng, lambda e: e.memset(ap, v), writes=[self.k(ap)])


def build_LA(S):
    NT = S // T
    nc = bass.Bass("TRN2", target_bir_lowering=False)
    hT = nc.dram_tensor("hT", [D, S], BF16, kind="ExternalInput").ap()
    wc = nc.dram_tensor("wc", [D, NCOL], F32, kind="ExternalInput").ap()
    pvd = nc.dram_tensor("pvd", [128, NPV], F32, kind="ExternalInput").ap()
    matsd = nc.dram_tensor("matsd", [128, NM, 128], F32, kind="ExternalInput").ap()
    cmd = nc.dram_tensor("cmd", [128, NCM, 512], F32, kind="ExternalInput").ap()
    yd = nc.dram_tensor("y", [3, 128, S], BF16, kind="ExternalOutput").ap()
    g = G(nc)
    b = Bld(g)
    wcb = g.sb([128, 16, NCOL], BF16, "wcb")
    pv = g.sb([128, NPV], F32, "pv")
    dv = g.sb([128, 8], F32, "dv")
    mats = g.sb([128, NM, 128], F32, "mats")
    cm = g.sb([128, NCM, 512], F32, "cm")
    hTt = [g.sb([128, 16, T], BF16, f"hTt{i}") for i in range(2)]
    P = {}
    for (nm, c0, rows, H) in GROUPS:
        P[nm] = [g.sb([128, H + T], F32, f"P_{nm}{i}") for i in range(2)]
    PJ = [g.ps([128, 512], F32, f"PJ{i}") for i in range(2)]
    M = [g.ps([128, 512], F32, f"M{i}") for i in range(6)]
    A = [g.sb([128, T], F32, f"A{i}") for i in range(22)]
    Bt = [g.sb([128, 512], F32, f"B{i}") for i in range(16)]
    HS = [g.sb([128, T], F32, f"HS{i}") for i in range(2)]
    YO = [[g.sb([128, T], BF16, f"Y{br}{i}") for i in range(2)] for br in range(3)]
    Sg = g.sb([128, 128], F32, "Sg")
    Sr = g.sb([128, 128], F32, "Sr")
    Usb = g.sb([64, 128], F32, "Usb")
    gcolT = g.sb([64, 8], F32, "gcolT")
    ident = mats[:, 5, :]
    ones = mats[:, 6, :]
    blk64 = mats[:, 7, :]
    RST, MSU, MSL, MUI, NMUI, IDR = range(6)

    def pcol(i):
        return pv[:, i:i + 1]

    for kq in range(4):
        g.dma("pool", wcb[:, 4 * kq:4 * kq + 4, :], wc.rearrange("(k p) n -> p k n", p=128)[:, 4 * kq:4 * kq + 4, :],
              writes=["wcb"], chan="wcb")
    g.dma("sp", pv[:], pvd, writes=["pv"], chan="pv")
    g.dma("sp", mats[:], matsd, writes=["mats"], chan="mats")
    g.dma("sp", cm[:], cmd, writes=["cm"], chan="cm")
    for nm in P:
        for i in range(2):
            b.memset(P[nm][i][:], 0.0)
    b.memset(Sg[:], 0.0)
    b.memset(Sr[:], 0.0)
    b.memset(HS[1][:], 0.0)
    b.act(dv[:, 0:1], pcol(7), AF.Exp, scale=-1.0)
    b.act(dv[:, 0:1], dv[:, 0:1], AF.Ln, bias=1.0)
    b.ts(dv[:, 0:1], dv[:, 0:1], -8.0, None, ALU.mult)
    b.act(dv[:, 1:2], pcol(20), AF.Exp)
    b.ts(dv[:, 1:2], dv[:, 1:2], -1.0, None, ALU.mult)
    b.ts(dv[:, 2:3], pcol(32), -1.0, 1.0, ALU.mult, ALU.add)

    hTv = hT.rearrange("(k p) s -> p k s", p=128)

    def load_h(t):
        g.dma("sp", hTt[t % 2][:], hTv[:, :, t * T:(t + 1) * T], writes=[f"hTt{t % 2}"], chan=f"hTt{t % 2}")

    def proj(t):
        par = t % 2
        for gi, (nm, c0, rows, H) in enumerate(GROUPS):
            pj = PJ[gi % 2]
            for kk in range(16):
                b.mm(pj[0:rows, 0:T], wcb[:, kk, c0:c0 + rows], hTt[par][:, kk, :], start=(kk == 0), stop=(kk == 15))
            dst = P[nm][par]
            b.cp(dst[0:rows, H:H + T], pj[0:rows, 0:T], eng="act")
            if H > 0 and t > 0:
                b.cp(dst[0:rows, 0:H], P[nm][1 - par][0:rows, T:T + H], eng="pool")

    def conv4(out, src, w0col, bias=None):
        if bias is None:
            b.ts(out, src[:, 0:T], pcol(w0col), None, ALU.mult)
        else:
            b.ts(out, src[:, 0:T], pcol(w0col), bias, ALU.mult, ALU.add)
        for j in range(1, 4):
            b.stt(out, src[:, j:j + T], pcol(w0col + j), out, ALU.mult, ALU.add)

    def inverse(N_, M_, R_, Pn, PTn, ncols, hs, psA, psB, psC):
        nb = ncols // 64
        b.tt(R_[0:64, 0:ncols], cm[0:64, IDR, 0:ncols], N_[0:64, 0:ncols], ALU.subtract)
        Pc, PTc = N_, M_
        for lev in range(1, 6):
            last = (lev == 5)
            Pnew, PTnew = (Pn, PTn) if lev % 2 == 1 else (N_, M_)
            for i in range(nb):
                sl = slice(i * 64, (i + 1) * 64)
                if not last:
                    b.mm(psA[0:64, sl], PTc[0:64, sl], Pc[0:64, sl])
                b.mm(psB[0:64, sl], Pc[0:64, sl], PTc[0:64, sl])
            if not last:
                b.cp(Pnew[0:64, 0:ncols], psA[0:64, 0:ncols], eng="act")
            b.cp(PTnew[0:64, 0:ncols], psB[0:64, 0:ncols], eng="dve")
            for i in range(nb):
                sl = slice(i * 64, (i + 1) * 64)
                b.mm(psC[0:64, sl], PTnew[0:64, sl], R_[0:64, sl])
            b.tt(R_[0:64, 0:ncols], R_[0:64, 0:ncols], psC[0:64, 0:ncols], ALU.add)
            Pc, PTc = Pnew, PTnew

    def lru(t):
        par = t % 2
        lx = P["lx"][par]
        lg = P["lg"][par]
        xc, r_, i_, m_ = A[0], A[1], A[2], A[3]
        conv4(xc[:], lx, 0, bias=pcol(4))
        b.mm(M[0][:, 0:T], mats[:, 0, :], xc[:])
        b.mm(M[1][:, 0:T], mats[:, 1, :], xc[:])
        b.act(r_[:], M[0][:, 0:T], AF.Sigmoid, bias=pcol(5))
        b.act(i_[:], M[1][:, 0:T], AF.Sigmoid, bias=pcol(6))
        b.act(r_[:], r_[:], AF.Exp, scale=dv[:, 0:1])
        b.act(m_[:], r_[:], AF.Square)
        b.act(m_[:], m_[:], AF.Sqrt, bias=1.0, scale=-1.0)
        b.tt(i_[:], i_[:], xc[:], ALU.mult)
        b.tt(i_[:], i_[:], m_[:], ALU.mult)
        b.scan(HS[par][:], r_[:], i_[:], HS[1 - par][:, T - 1:T])
        gsc = A[0]
        b.act(gsc[:], lg[:, 0:T], AF.Square)
        b.ts(gsc[:], gsc[:], 0.044715, 1.0, ALU.mult, ALU.add)
        b.tt(gsc[:], gsc[:], lg[:, 0:T], ALU.mult)
        b.act(gsc[:], gsc[:], AF.Sigmoid, scale=1.5957691216057308)
        b.tt(gsc[:], gsc[:], lg[:, 0:T], ALU.mult)
        b.tt(YO[0][par][:], gsc[:], HS[par][:], ALU.mult)
        g.dma("sp", yd[0, :, t * T:(t + 1) * T], YO[0][par][:], reads=[f"Y0{par}"], chan=f"yo0{par}")

    def gdn(t):
        par = t % 2
        q_, k_, v_ = A[4], A[5], A[6]
        tmp, beta_b, gb, gc_b, egc, kd, Kbeta, Kbe, qd = A[7], A[8], A[9], A[10], A[11], A[12], A[13], A[14], A[15]
        conv4(q_[:], P["q"][par], 8)
        conv4(k_[:], P["k"][par], 12)
        conv4(v_[:], P["v"][par], 16)
        for x_ in (q_, k_, v_):
            b.act(x_[:], x_[:], AF.Silu)
        for x_, sc in ((q_, 128.0 ** -0.5), (k_, 1.0)):
            b.act(tmp[:], x_[:], AF.Square)
            b.mm(M[0][:, 0:T], ones, tmp[:])
            b.act(tmp[:], M[0][:, 0:T], AF.Sqrt, bias=EPS)
            b.recip(tmp[:], tmp[:])
            b.stt(x_[:], x_[:], sc, tmp[:], ALU.mult, ALU.mult)
        ba = P["ba"][par]
        b.mm(M[0][:, 0:T], mats[0:2, 8, :], ba[0:2, 0:T])
        b.mm(M[1][:, 0:T], mats[0:2, 9, :], ba[0:2, 0:T])
        b.act(beta_b[:], M[0][:, 0:T], AF.Sigmoid)
        b.act(gb[:], M[1][:, 0:T], AF.Exp, bias=pcol(21))
        b.act(gb[:], gb[:], AF.Ln, bias=1.0)
        b.ts(gb[:], gb[:], dv[:, 1:2], None, ALU.mult)
        b.scan(gc_b[:], cm[:, RST, 0:T], gb[:], 0.0)
        for n in range(NCH):
            sl = slice(n * C, (n + 1) * C)
            b.ts(kd[:, sl], gc_b[:, sl], gc_b[:, n * C + C - 1:n * C + C], None, ALU.subtract)
        b.act(kd[:], kd[:], AF.Exp, scale=-1.0)
        b.act(egc[:], gc_b[:], AF.Exp)
        b.tt(kd[:], kd[:], k_[:], ALU.mult)
        b.tt(Kbeta[:], k_[:], beta_b[:], ALU.mult)
        b.tt(Kbe[:], Kbeta[:], egc[:], ALU.mult)
        b.tt(v_[:], v_[:], beta_b[:], ALU.mult)
        b.tt(qd[:], q_[:], egc[:], ALU.mult)
        for n in range(NCH):
            b.tr(M[2][0:64, n * 32:(n + 1) * 32], gc_b[0:32, n * C:(n + 1) * C], ident[0:32, 0:32])
        b.cp(gcolT[:, 0:NCH], M[2][0:64, 0:NCH * 32:32])
        DT_, D_, DTsu, R_, Pn, PTn = Bt[0], Bt[1], Bt[2], Bt[3], Bt[4], Bt[5]
        for n in range(NCH):
            sl = slice(n * C, (n + 1) * C)
            b.ts(DT_[0:64, sl], gc_b[0:64, sl], gcolT[:, n:n + 1], 0.0, ALU.subtract, ALU.min)
            b.ts(D_[0:64, sl], gc_b[0:64, sl], gcolT[:, n:n + 1], 0.0, ALU.subtract, ALU.max)
        b.act(DT_[0:64, 0:T], DT_[0:64, 0:T], AF.Exp)
        b.act(D_[0:64, 0:T], D_[0:64, 0:T], AF.Exp, scale=-1.0)
        b.tt(DTsu[0:64, 0:T], DT_[0:64, 0:T], cm[0:64, MSU, 0:T], ALU.mult)
        b.tt(D_[0:64, 0:T], D_[0:64, 0:T], cm[0:64, MSL, 0:T], ALU.mult)
        b.tt(DT_[0:64, 0:T], DT_[0:64, 0:T], cm[0:64, MUI, 0:T], ALU.mult)
        for n in range(NCH):
            sl = slice(n * C, (n + 1) * C)
            b.mm(M[0][0:64, sl], k_[:, sl], Kbeta[:, sl])
            b.mm(M[1][0:64, sl], Kbeta[:, sl], k_[:, sl])
            b.mm(M[2][0:64, sl], k_[:, sl], q_[:, sl])
        b.tt(DTsu[0:64, 0:T], DTsu[0:64, 0:T], M[0][0:64, 0:T], ALU.mult)
        b.tt(D_[0:64, 0:T], D_[0:64, 0:T], M[1][0:64, 0:T], ALU.mult)
        b.tt(DT_[0:64, 0:T], DT_[0:64, 0:T], M[2][0:64, 0:T], ALU.mult)
        inverse(DTsu, D_, R_, Pn, PTn, T, None, M[0], M[1], M[2])
        Kbe_tm, Vb_tm, Kd_tm, U0, WkT = Bt[6], Bt[7], Bt[8], Bt[9], A[13]
        for n in range(NCH):
            sl = slice(n * C, (n + 1) * C)
            b.tr(M[3][0:64, n * 128:(n + 1) * 128], Kbe[:, sl], ident)
            b.tr(M[4][0:64, n * 128:(n + 1) * 128], v_[:, sl], ident)
            b.tr(M[5][0:64, n * 128:(n + 1) * 128], kd[:, sl], ident)
        b.cp(Kbe_tm[0:64, :], M[3][0:64, :], eng="act")
        b.cp(Vb_tm[0:64, :], M[4][0:64, :], eng="dve")
        b.cp(Kd_tm[0:64, :], M[5][0:64, :], eng="act")
        for n in range(NCH):
            sl = slice(n * C, (n + 1) * C)
            b.mm(M[0][0:64, n * 128:(n + 1) * 128], R_[0:64, sl], Vb_tm[0:64, n * 128:(n + 1) * 128])
            b.mm(M[1][:, sl], Kbe_tm[0:64, n * 128:(n + 1) * 128], R_[0:64, sl])
        b.cp(U0[0:64, :], M[0][0:64, :], eng="dve")
        b.cp(WkT[:], M[1][:, 0:T], eng="act")
        for n in range(NCH):
            sl = slice(n * C, (n + 1) * C)
            b.mm(M[2][0:64, 0:128], WkT[:, sl], Sg[:, :])
            b.tt(Usb[:, :], U0[0:64, n * 128:(n + 1) * 128], M[2][0:64, 0:128], ALU.subtract)
            b.mm(M[3][:, sl], Sg[:, :], qd[:, sl], start=True, stop=False)
            b.mm(M[3][:, sl], Usb[:, :], DT_[0:64, sl], start=False, stop=True)
            b.mm(M[4][:, 0:128], Kd_tm[0:64, n * 128:(n + 1) * 128], Usb[:, :])
            b.stt(Sg[:, :], Sg[:, :], egc[:, n * C + C - 1:n * C + C], M[4][:, 0:128], ALU.mult, ALU.add)
        osb, sz = A[7], A[8]
        b.act(tmp[:], M[3][:, 0:T], AF.Square)
        b.mm(M[0][:, 0:T], ones, tmp[:])
        b.act(tmp[:], M[0][:, 0:T], AF.Sqrt, bias=EPS, scale=1.0 / 128)
        b.recip(tmp[:], tmp[:])
        b.tt(tmp[:], tmp[:], M[3][:, 0:T], ALU.mult)
        b.act(sz[:], P["z"][par][:, 0:T], AF.Silu)
        b.stt(YO[1][par][:], tmp[:], pcol(22), sz[:], ALU.mult, ALU.mult)
        g.dma("sp", yd[1, :, t * T:(t + 1) * T], YO[1][par][:], reads=[f"Y1{par}"], chan=f"yo1{par}")

    def rwkv(t):
        par = t % 2
        r_, k_, v_, wa_, g1_, g2_ = A[0], A[1], A[2], A[3], A[4], A[5]
        tmp = A[6]
        for dst, nm, mucol, rows in ((r_, "r", 23, 128), (k_, "rk", 24, 128), (v_, "rv", 25, 128), (wa_, "wa", 26, 128),
                                     (g1_, "g1", 27, 128), (g2_, "g2", 28, 32)):
            src = P[nm][par]
            b.tt(tmp[0:rows, :], src[0:rows, 0:T], src[0:rows, 1:T + 1], ALU.subtract)
            b.stt(dst[0:rows, :], tmp[0:rows, :], pv[0:rows, mucol:mucol + 1], src[0:rows, 1:T + 1], ALU.mult, ALU.add)
        ld, a_, g_, kk, p_, cum = A[7], A[8], A[9], A[10], A[11], A[12]
        b.act(tmp[0:64, :], wa_[0:64, :], AF.Tanh)
        b.mm(M[0][:, 0:T], mats[0:64, 2, :], tmp[0:64, :])
        b.act(ld[:], M[0][:, 0:T], AF.Sigmoid, bias=pcol(29))
        b.ts(ld[:], ld[:], -0.6065306597126334, None, ALU.mult)
        b.mm(M[1][:, 0:T], mats[64:128, 2, :], wa_[64:128, :])
        b.act(a_[:], M[1][:, 0:T], AF.Sigmoid, bias=pcol(30))
        b.act(g1_[:], g1_[:], AF.Sigmoid)
        b.act(g2_[0:32, :], g2_[0:32, :], AF.Sigmoid)
        b.mm(M[2][:, 0:T], mats[:, 3, :], g1_[:], start=True, stop=False)
        b.mm(M[2][:, 0:T], mats[0:32, 4, :], g2_[0:32, :], start=False, stop=True)
        b.cp(g_[:], M[2][:, 0:T], eng="act")
        b.ts(kk[:], k_[:], pcol(31), None, ALU.mult)
        b.act(tmp[:], kk[:], AF.Square)
        b.mm(M[0][:, 0:T], blk64, tmp[:])
        b.act(tmp[:], M[0][:, 0:T], AF.Sqrt, bias=EPS)
        b.recip(tmp[:], tmp[:])
        b.tt(kk[:], kk[:], tmp[:], ALU.mult)
        b.ts(tmp[:], a_[:], pcol(32), dv[:, 2:3], ALU.mult, ALU.add)
        b.tt(k_[:], k_[:], tmp[:], ALU.mult)
        b.tt(p_[:], kk[:], a_[:], ALU.mult)
        rkb = A[13]
        b.stt(rkb[:], r_[:], pcol(33), k_[:], ALU.mult, ALU.mult)
        b.scan(cum[:], cm[:, RST, 0:T], ld[:], 0.0)
        ecum, encum, ecx, ecl = A[14], A[15], A[16], A[17]
        b.act(ecum[:], cum[:], AF.Exp)
        b.act(encum[:], cum[:], AF.Exp, scale=-1.0)
        b.tt(ecx[:], cum[:], ld[:], ALU.subtract)
        b.act(ecx[:], ecx[:], AF.Exp)
        for n in range(NCH):
            sl = slice(n * C, (n + 1) * C)
            b.ts(ecl[:, sl], cum[:, sl], cum[:, n * C + C - 1:n * C + C], None, ALU.subtract)
        b.act(ecl[:], ecl[:], AF.Exp, scale=-1.0)
        Rh, Kt, Pt, KKh, Kbar, Pbar = A[18], A[19], A[20], A[21], A[6], A[7]
        b.tt(Rh[:], r_[:], ecum[:], ALU.mult)
        b.tt(Kt[:], k_[:], encum[:], ALU.mult)
        b.tt(Pt[:], p_[:], encum[:], ALU.mult)
        b.tt(KKh[:], kk[:], ecx[:], ALU.mult)
        b.tt(Kbar[:], k_[:], ecl[:], ALU.mult)
        b.tt(Pbar[:], p_[:], ecl[:], ALU.mult)
        N_, M_, AkvT, ArkT, nArpT, R_, Pn, PTn = Bt[0], Bt[1], Bt[2], Bt[3], Bt[4], Bt[5], Bt[10], Bt[11]

        def blk(h, n):
            return slice((h * NCH + n) * 64, (h * NCH + n + 1) * 64)

        specs = [(N_, Pt, KKh, MSU), (M_, KKh, Pt, MSL), (AkvT, Kt, KKh, MSU), (ArkT, Kt, Rh, MUI), (nArpT, Pt, Rh, NMUI)]
        for si, (dst, lt, rt, msk) in enumerate(specs):
            for h in range(2):
                hp = slice(64 * h, 64 * h + 64)
                psb = M[(2 * si + h) % 6]
                for n in range(NCH):
                    sl = slice(n * C, (n + 1) * C)
                    b.mm(psb[0:64, sl], lt[hp, sl], rt[hp, sl])
                b.tt(dst[0:64, h * T:(h + 1) * T], psb[0:64, 0:T], cm[0:64, msk, 0:T], ALU.mult)
        inverse(N_, M_, R_, Pn, PTn, 512, None, M[0], M[1], M[2])
        V_tm, KK_tm, Kb_tm, nPb_tm = Bt[6], Bt[7], Bt[8], Bt[9]
        for n in range(NCH):
            sl = slice(n * C, (n + 1) * C)
            b.tr(M[0][0:64, n * 128:(n + 1) * 128], v_[:, sl], ident)
            b.tr(M[1][0:64, n * 128:(n + 1) * 128], KKh[:, sl], ident)
            b.tr(M[2][0:64, n * 128:(n + 1) * 128], Kbar[:, sl], ident)
            b.tr(M[3][0:64, n * 128:(n + 1) * 128], Pbar[:, sl], ident)
        b.cp(V_tm[0:64, :], M[0][0:64, :], eng="act")
        b.cp(KK_tm[0:64, :], M[1][0:64, :], eng="dve")
        b.cp(Kb_tm[0:64, :], M[2][0:64, :], eng="act")
        b.ts(nPb_tm[0:64, :], M[3][0:64, :], -1.0, None, ALU.mult)
        Y_, U0, W1T = Bt[12], Bt[13], A[8]

        def tm(n, h):
            return slice(n * 128 + h * 64, n * 128 + h * 64 + 64)

        for n in range(NCH):
            for h in range(2):
                b.mm(M[4][0:64, tm(n, h)], AkvT[0:64, blk(h, n)], V_tm[0:64, tm(n, h)])
        b.cp(Y_[0:64, :], M[4][0:64, :], eng="dve")
        for n in range(NCH):
            for h in range(2):
                b.mm(M[5][0:64, tm(n, h)], R_[0:64, blk(h, n)], Y_[0:64, tm(n, h)])
                b.mm(M[0][64 * h:64 * h + 64, n * C:(n + 1) * C], KK_tm[0:64, tm(n, h)], R_[0:64, blk(h, n)])
        b.cp(U0[0:64, :], M[5][0:64, :], eng="dve")
        b.cp(W1T[:], M[0][:, 0:T], eng="act")
        for n in range(NCH):
            sl = slice(n * C, (n + 1) * C)
            b.mm(M[1][0:64, 0:128], W1T[:, sl], Sr[:, :])
            b.tt(Usb[:, :], U0[0:64, n * 128:(n + 1) * 128], M[1][0:64, 0:128], ALU.add)
            b.mm(M[2][:, sl], Sr[:, :], Rh[:, sl], start=True, stop=False)
            for h in range(2):
                hp = slice(64 * h, 64 * h + 64)
                last = (h == 1)
                b.mm(M[2][hp, sl], V_tm[0:64, tm(n, h)], ArkT[0:64, blk(h, n)], start=False, stop=False)
                b.mm(M[2][hp, sl], Usb[:, 64 * h:64 * h + 64], nArpT[0:64, blk(h, n)], start=False, stop=True)
                b.mm(M[3][hp, 64 * h:64 * h + 64], Kb_tm[0:64, tm(n, h)], V_tm[0:64, tm(n, h)], start=True, stop=False)
                b.mm(M[3][hp, 64 * h:64 * h + 64], nPb_tm[0:64, tm(n, h)], Usb[:, 64 * h:64 * h + 64], start=False, stop=True)
            for h in range(2):
                hp = slice(64 * h, 64 * h + 64)
                hc = slice(64 * h, 64 * h + 64)
                b.stt(Sr[hp, hc], Sr[hp, hc], ecum[hp, n * C + C - 1:n * C + C], M[3][hp, hc], ALU.mult, ALU.add)
        osb, cen = A[9 + 1], A[11]
        b.cp(osb[:], M[2][:, 0:T], eng="act")
        b.mm(M[4][:, 0:T], blk64, osb[:])
        b.stt(cen[:], M[4][:, 0:T], -1.0 / 64, osb[:], ALU.mult, ALU.add)
        b.act(tmp[:], cen[:], AF.Square)
        b.mm(M[5][:, 0:T], blk64, tmp[:])
        b.act(tmp[:], M[5][:, 0:T], AF.Sqrt, bias=64e-5, scale=1.0 / 64)
        b.recip(tmp[:], tmp[:])
        b.tt(cen[:], cen[:], tmp[:], ALU.mult)
        b.ts(cen[:], cen[:], pcol(34), pcol(35), ALU.mult, ALU.add)
        b.mm(M[4][:, 0:T], blk64, rkb[:])
        b.tt(tmp[:], M[4][:, 0:T], v_[:], ALU.mult)
        b.tt(cen[:], cen[:], tmp[:], ALU.add)
        b.tt(YO[2][par][:], cen[:], g_[:], ALU.mult)
        g.dma("sp", yd[2, :, t * T:(t + 1) * T], YO[2][par][:], reads=[f"Y2{par}"], chan=f"yo2{par}")

    branches = "abc"
    load_h(0)
    proj(0)
    for t in range(NT):
        if t + 1 < NT:
            load_h(t + 1)
            proj(t + 1)
        if "a" in branches:
            lru(t)
        if "b" in branches:
            gdn(t)
        if "c" in branches:
            rwkv(t)
    g.emit()
    return nc


def la_cols(c):
    cols = []
    for base in (0, 1024, 2048, 3072, 4096, 5120, 6160, 7184, 8208):
        cols += list(range(base + c * 128, base + c * 128 + 128))
    cols += list(range(9232, 9360))
    cols += list(range(9360, 9520))
    cols += [6144 + c, 6152 + c]
    return np.array(cols)


def la_consts():
    cmv = np.zeros((128, NCM, 512), np.float32)
    j = np.arange(64)[:, None]
    i = np.arange(512)[None, :] % 64
    cmv[:, 0, :] = 1.0
    cmv[:, 0, ::64] = 0.0
    cmv[0:64, 1, :] = (j < i)
    cmv[0:64, 2, :] = (j > i)
    cmv[0:64, 3, :] = (j <= i)
    cmv[0:64, 4, :] = -1.0 * (j <= i)
    cmv[0:64, 5, :] = (j == i)
    return cmv


def la_inputs(inp, l, c):
    f = np.float32
    sl = slice(c * 128, (c + 1) * 128)
    pv = np.zeros((128, NPV), f)
    pv[:, 0:4] = inp["lru_conv_w"][l][:, sl].T
    pv[:, 4] = inp["lru_conv_b"][l][sl]
    pv[:, 5] = inp["lru_b_a"][l][sl]
    pv[:, 6] = inp["lru_b_x"][l][sl]
    pv[:, 7] = inp["lru_lambda"][l][sl]
    gw = inp["gdn_conv_w"][l]
    pv[:, 8:12] = gw[:, c * 128:(c + 1) * 128].T
    pv[:, 12:16] = gw[:, 1024 + c * 128:1024 + (c + 1) * 128].T
    pv[:, 16:20] = gw[:, 2048 + c * 128:2048 + (c + 1) * 128].T
    pv[:, 20] = inp["gdn_a_log"][l][c]
    pv[:, 21] = inp["gdn_dt_bias"][l][c]
    pv[:, 22] = inp["gdn_norm_w"][l]
    mu = inp["rwkv_mu"][l]
    pv[:, 23] = mu[c * 128:(c + 1) * 128]
    pv[:, 24] = mu[1024 + c * 128:1024 + (c + 1) * 128]
    pv[:, 25] = mu[2048 + c * 128:2048 + (c + 1) * 128]
    pv[:, 26] = mu[3072:3200]
    pv[:, 27] = mu[3200:3328]
    pv[0:32, 28] = mu[3328:3360]
    pv[:, 29] = inp["rwkv_w0"][l][sl]
    pv[:, 30] = inp["rwkv_a0"][l][sl]
    pv[:, 31] = inp["rwkv_k_k"][l][sl]
    pv[:, 32] = inp["rwkv_k_a"][l][sl]
    pv[:, 33] = inp["rwkv_r_k"][l].reshape(-1)[sl]
    pv[:, 34] = inp["rwkv_ln_w"][l][sl]
    pv[:, 35] = inp["rwkv_ln_b"][l][sl]
    mats = np.zeros((128, NM, 128), f)
    for gl in range(2):
        mats[gl * 64:(gl + 1) * 64, 0, gl * 64:(gl + 1) * 64] = inp["lru_w_a"][l][2 * c + gl]
        mats[gl * 64:(gl + 1) * 64, 1, gl * 64:(gl + 1) * 64] = inp["lru_w_x"][l][2 * c + gl]
    mats[0:64, 2, :] = inp["rwkv_w_up"][l][:, sl]
    mats[64:128, 2, :] = inp["rwkv_a_up"][l][:, sl]
    mats[:, 3, :] = inp["rwkv_g_up"][l][0:128, sl]
    mats[0:32, 4, :] = inp["rwkv_g_up"][l][128:160, sl]
    mats[:, 5, :] = np.eye(128, dtype=f)
    mats[:, 6, :] = 1.0
    mats[0:64, 7, 0:64] = 1.0
    mats[64:128, 7, 64:128] = 1.0
    mats[0, 8, :] = 1.0
    mats[1, 9, :] = 1.0
    wc = np.ascontiguousarray(inp["w_in"][l][:, la_cols(c)])
    return dict(wc=wc, pvd=pv, matsd=mats, cmd=la_consts())


TB = 512
DFF = 5632
NF = DFF // 128


def build_LB(NTOK, mode="full"):
    NTL = NTOK // TB
    nc = bass.Bass("TRN2", target_bir_lowering=False)
    xTd = nc.dram_tensor("xT", [D, NTOK], F32, kind="ExternalInput").ap()
    nvd = nc.dram_tensor("nvd", [128, 16, 4], F32, kind="ExternalInput").ap()
    onesd = nc.dram_tensor("onesd", [128, 128], F32, kind="ExternalInput").ap()
    hTo = nc.dram_tensor("hT_out", [D, NTOK], BF16, kind="ExternalOutput").ap()
    full = (mode == "full")
    if full:
        yTd = nc.dram_tensor("yT", [3072, NTOK], BF16, kind="ExternalInput").ap()
        hTd = nc.dram_tensor("hT", [D, NTOK], BF16, kind="ExternalInput").ap()
        wg3 = nc.dram_tensor("wg3", [D, 6144], F32, kind="ExternalInput").ap()
        wbr = nc.dram_tensor("wbr", [3072, D], F32, kind="ExternalInput").ap()
        wo = nc.dram_tensor("wo", [D, D], F32, kind="ExternalInput").ap()
        wfg = nc.dram_tensor("wfg", [D, DFF], F32, kind="ExternalInput").ap()
        wfu = nc.dram_tensor("wfu", [D, DFF], F32, kind="ExternalInput").ap()
        wfd = nc.dram_tensor("wfd", [DFF, D], F32, kind="ExternalInput").ap()
        xTo = nc.dram_tensor("xT_out", [D, NTOK], F32, kind="ExternalOutput").ap()
    g = G(nc)
    b = Bld(g)
    nv = g.sb([128, 16, 4], F32, "nv")
    ones = g.sb([128, 128], F32, "ones")
    xt = g.sb([128, 16, TB], F32, "xt")
    hb = g.sb([128, 16, TB], BF16, "hb")
    rstd = g.sb([128, TB], F32, "rstd")
    sq = [g.sb([128, TB], F32, f"sq{i}") for i in range(2)]
    SS = g.ps([128, 512], F32, "SS")
    g.dma("sp", nv[:], nvd, writes=["nv"], chan="nv")
    g.dma("sp", ones[:], onesd, writes=["ones"], chan="ones")
    if full:
        big = g.sb([128, NF * TB], BF16, "big")
        yt = big[:, 0:24 * TB].rearrange("p (k t) -> p k t", t=TB)
        mixT = big[:, 24 * TB:40 * TB].rearrange("p (k t) -> p k t", t=TB)
        aT = big[:, :].rearrange("p (k t) -> p k t", t=TB)
        oT = g.sb([128, 16, TB], F32, "oT")
        WP = [g.sb([128, 12288], BF16, f"WP{i}") for i in range(2)]
        gsb = [g.sb([128, TB], F32, f"gsb{i}") for i in range(2)]
        tmpb = [g.sb([128, TB], F32, f"tmpb{i}") for i in range(2)]
        macc = g.sb([128, 4, TB], F32, "macc")
        pA = [g.ps([128, 512], F32, f"pA{i}") for i in range(2)]
        pB = [g.ps([128, 512], F32, f"pB{i}") for i in range(2)]

    xTv = xTd.rearrange("(k p) s -> p k s", p=128)
    hTov = hTo.rearrange("(k p) s -> p k s", p=128)

    def norm_stats_finish():
        b.act(rstd[:], SS[:, 0:TB], AF.Sqrt, bias=EPS, scale=1.0 / D)
        b.recip(rstd[:], rstd[:])

    def sumsq(src_fn):
        for k in range(16):
            s_ = sq[k % 2]
            b.act(s_[:], src_fn(k), AF.Square)
            b.mm(SS[:, 0:TB], ones[:, :], s_[:], start=(k == 0), stop=(k == 15))
        norm_stats_finish()

    def residual_add(j):
        for k in range(16):
            t_ = sq[k % 2]
            b.stt(t_[:], oT[:, k, :], nv[:, k, j:j + 1], rstd[:], ALU.mult, ALU.mult)
            b.tt(xt[:, k, :], xt[:, k, :], t_[:], ALU.add)

    def norm_to(dst, j):
        sumsq(lambda k: xt[:, k, :])
        for k in range(16):
            b.stt(dst[:, k, :], xt[:, k, :], nv[:, k, j:j + 1], rstd[:], ALU.mult, ALU.mult)

    items = []

    def wview(buf, off, K, n):
        return buf[:, off:off + K * n].rearrange("p (k n) -> p k n", n=n)

    for tl in range(NTL):
        tsl = slice(tl * TB, (tl + 1) * TB)

        def start_tile(buf, tsl=tsl):
            g.dma("sp", xt[:], xTv[:, :, tsl], writes=["xt"], chan="xt")
            if full:
                g.dma("sp", hb[:], hTd.rearrange("(k p) s -> p k s", p=128)[:, :, tsl], writes=["hb"], chan="hb")
                g.dma("sp", yt, yTd.rearrange("(k p) s -> p k s", p=128)[:, :, tsl], writes=["big"], chan="big")

        if not full:
            def only_norm(buf, tsl=tsl, st=start_tile):
                st(None)
                norm_to(hb, 3)
                g.dma("sp", hTov[:, :, tsl], hb[:], reads=["hb"], chan="hbo")
            items.append((None, only_norm))
            continue

        for dcg in range(4):
            for br in range(3):
                def ld(buf, dcg=dcg, br=br):
                    g.dma("pool", wview(buf, 0, 16, 512), wg3.rearrange("(k p) n -> p k n", p=128)[:, :, br * 2048 + dcg * 512: br * 2048 + dcg * 512 + 512],
                          writes=[buf.name], chan=buf.name)
                    g.dma("pool", wview(buf, 8192, 8, 512), wbr.rearrange("(k p) n -> p k n", p=128)[:, br * 8:br * 8 + 8, dcg * 512:dcg * 512 + 512],
                          writes=[buf.name], chan=buf.name)

                def cmp(buf, dcg=dcg, br=br, first=(dcg == 0 and br == 0), tsl=tsl, st=start_tile):
                    if first:
                        st(None)
                    Wg = wview(buf, 0, 16, 512)
                    Wb = wview(buf, 8192, 8, 512)
                    for dci in range(4):
                        dc = dcg * 4 + dci
                        cs = slice(dci * 128, dci * 128 + 128)
                        pg, pb = pA[dci % 2], pB[dci % 2]
                        for k in range(16):
                            b.mm(pg[:, 0:TB], Wg[:, k, cs], hb[:, k, :], start=(k == 0), stop=(k == 15))
                        b.act(gsb[dci % 2][:], pg[:, 0:TB], AF.Sigmoid)
                        for k in range(8):
                            b.mm(pb[:, 0:TB], Wb[:, k, cs], yt[:, br * 8 + k, :], start=(k == 0), stop=(k == 7))
                        if br == 0:
                            b.tt(macc[:, dci, :], gsb[dci % 2][:], pb[:, 0:TB], ALU.mult)
                        elif br == 1:
                            b.tt(tmpb[dci % 2][:], gsb[dci % 2][:], pb[:, 0:TB], ALU.mult)
                            b.tt(macc[:, dci, :], macc[:, dci, :], tmpb[dci % 2][:], ALU.add)
                        else:
                            b.tt(tmpb[dci % 2][:], gsb[dci % 2][:], pb[:, 0:TB], ALU.mult)
                            b.tt(mixT[:, dc, :], macc[:, dci, :], tmpb[dci % 2][:], ALU.add)
                items.append((ld, cmp))

        def dense_items(w_ap, K, ncols_total, pcols, rhs_fn, evac_fn, post_fn=None):
            npan = ncols_total // pcols
            for pi in range(npan):
                def ld(buf, pi=pi):
                    g.dma("pool", wview(buf, 0, K, pcols), w_ap.rearrange("(k p) n -> p k n", p=128)[:, :, pi * pcols:(pi + 1) * pcols],
                          writes=[buf.name], chan=buf.name)

                def cmp(buf, pi=pi, lastp=(pi == npan - 1)):
                    W = wview(buf, 0, K, pcols)
                    for ci in range(pcols // 128):
                        dc = pi * (pcols // 128) + ci
                        ps_ = pA[dc % 2]
                        for k in range(K):
                            b.mm(ps_[:, 0:TB], W[:, k, ci * 128:(ci + 1) * 128], rhs_fn(k), start=(k == 0), stop=(k == K - 1))
                        evac_fn(dc, ps_)
                    if lastp and post_fn is not None:
                        post_fn()
                items.append((ld, cmp))

        def evac_o(dc, ps_):
            b.cp(oT[:, dc, :], ps_[:, 0:TB], eng="act")
            s_ = sq[dc % 2]
            b.act(s_[:], ps_[:, 0:TB], AF.Square)
            b.mm(SS[:, 0:TB], ones[:, :], s_[:], start=(dc == 0), stop=(dc == 15))

        def post_mix():
            norm_stats_finish()
            residual_add(0)
            norm_to(hb, 1)

        def post_ffn(tsl=tsl):
            norm_stats_finish()
            residual_add(2)
            g.dma("sp", xTo.rearrange("(k p) s -> p k s", p=128)[:, :, tsl], xt[:], reads=["xt"], chan="xto")
            norm_to(hb, 3)
            g.dma("sp", hTov[:, :, tsl], hb[:], reads=["hb"], chan="hbo")

        dense_items(wo, 16, D, 512, lambda k: mixT[:, k, :], evac_o, post_mix)
        for fi in range(DFF // 256):
            def ld(buf, fi=fi):
                g.dma("pool", wview(buf, 0, 16, 256), wfg.rearrange("(k p) n -> p k n", p=128)[:, :, fi * 256:fi * 256 + 256], writes=[buf.name], chan=buf.name)
                g.dma("pool", wview(buf, 4096, 16, 256), wfu.rearrange("(k p) n -> p k n", p=128)[:, :, fi * 256:fi * 256 + 256], writes=[buf.name], chan=buf.name)

            def cmp(buf, fi=fi):
                Wg_ = wview(buf, 0, 16, 256)
                Wu_ = wview(buf, 4096, 16, 256)
                for ci in range(2):
                    fc = fi * 2 + ci
                    pg, pu = pA[ci], pB[ci]
                    for k in range(16):
                        b.mm(pg[:, 0:TB], Wg_[:, k, ci * 128:(ci + 1) * 128], hb[:, k, :], start=(k == 0), stop=(k == 15))
                    for k in range(16):
                        b.mm(pu[:, 0:TB], Wu_[:, k, ci * 128:(ci + 1) * 128], hb[:, k, :], start=(k == 0), stop=(k == 15))
                    b.act(gsb[ci][:], pg[:, 0:TB], AF.Silu)
                    b.tt(aT[:, fc, :], gsb[ci][:], pu[:, 0:TB], ALU.mult)
            items.append((ld, cmp))
        dense_items(wfd, NF, D, 256, lambda k: aT[:, k, :], evac_o, post_ffn)

    nb = 0
    bufs = []
    for it in items:
        bufs.append(None)
    def buf_of(i):
        return WP[i % 2] if full else None
    if items and items[0][0] is not None:
        items[0][0](buf_of(0))
    for i, (ld, cmp) in enumerate(items):
        if i + 1 < len(items) and items[i + 1][0] is not None:
            items[i + 1][0](buf_of(i + 1))
        cmp(buf_of(i))
    g.emit()
    return nc


def lb_nv(inp, l):
    nv = np.zeros((128, 16, 4), np.float32)
    vecs = [inp["norm_mix_post"][l], inp["norm_ffn_pre"][l], inp["norm_ffn_post"][l],
            inp["norm_mix_pre"][l + 1] if l + 1 < inp["norm_mix_pre"].shape[0] else np.ones(D, np.float32)]
    for j, v in enumerate(vecs):
        nv[:, :, j] = v.reshape(16, 128).T
    return nv


def _run(nc, maps):
    return run_bass_kernel_spmd(nc, maps, core_ids=list(range(len(maps)))).results


def kernel(**inputs):
    inp = {k: np.asarray(v) for k, v in inputs.items()}
    f32 = np.float32
    NCORE = 8
    x = inp["x"][0].astype(f32)
    S = x.shape[0]
    NTOK = S // NCORE
    L = inp["w_in"].shape[0]
    ones = np.ones((128, 128), f32)
    xT_sh = [np.ascontiguousarray(x[c * NTOK:(c + 1) * NTOK].T) for c in range(NCORE)]
    nv0 = np.zeros((128, 16, 4), f32)
    nv0[:, :, 3] = inp["norm_mix_pre"][0].reshape(16, 128).T
    nc0 = build_LB(NTOK, "norm")
    r0 = _run(nc0, [dict(xT=xT_sh[c], nvd=nv0, onesd=ones) for c in range(NCORE)])
    hT_sh = [r0[c]["hT_out"] for c in range(NCORE)]
    ncA = build_LA(S)
    ncB = build_LB(NTOK, "full")
    for l in range(L):
        hT_all = np.ascontiguousarray(np.concatenate(hT_sh, axis=1))
        mapsA = []
        for c in range(NCORE):
            m = la_inputs(inp, l, c)
            m["hT"] = hT_all
            mapsA.append(m)
        rA = _run(ncA, mapsA)
        yfull = np.stack([rA[c]["y"] for c in range(NCORE)], axis=1).reshape(3072, S)
        del rA, mapsA
        wg3 = np.ascontiguousarray(inp["w_in"][l][:, 9520:])
        wbr = np.ascontiguousarray(inp["w_branch"][l].reshape(3072, D))
        nvl = lb_nv(inp, l)
        mapsB = [dict(xT=xT_sh[c], yT=np.ascontiguousarray(yfull[:, c * NTOK:(c + 1) * NTOK]), hT=hT_sh[c],
                      nvd=nvl, onesd=ones, wg3=wg3, wbr=wbr, wo=inp["w_out"][l], wfg=inp["ffn_w_gate"][l],
                      wfu=inp["ffn_w_up"][l], wfd=inp["ffn_w_down"][l]) for c in range(NCORE)]
        rB = _run(ncB, mapsB)
        xT_sh = [rB[c]["xT_out"] for c in range(NCORE)]
        hT_sh = [rB[c]["hT_out"] for c in range(NCORE)]
        del rB, mapsB
    out = np.concatenate([np.asarray(xT_sh[c], dtype=f32).T for c in range(NCORE)], axis=0)
    return np.ascontiguousarray(out[None]).astype(f32)
```

```python
import ml_dtypes
import numpy as np
from contextlib import ExitStack
import concourse.bass as bass
import concourse.mybir as mybir
from concourse.bass_utils import run_bass_kernel_spmd

F32 = mybir.dt.float32
BF16 = mybir.dt.bfloat16
ALU = mybir.AluOpType
AF = mybir.ActivationFunctionType

ENGS = ("pe", "act", "dve", "pool", "sp")


class _Op:
    __slots__ = ("eng", "fn", "deps", "dma", "chan", "cnt", "inc", "idx")

    def __init__(self, eng, fn, dma, chan):
        self.eng = eng
        self.fn = fn
        self.deps = []
        self.dma = dma
        self.chan = chan
        self.cnt = 0
        self.inc = False
        self.idx = 0


class G:
    def __init__(self, nc):
        self.nc = nc
        self.ops = {e: [] for e in ENGS}
        self.lastw = {}
        self.rds = {}
        self.chan_cnt = {}
        self.stack = ExitStack()
        self.n_sb = 0

    def sb(self, shape, dt=F32, name=None):
        self.n_sb += 1
        return self.stack.enter_context(self.nc.sbuf_tensor(name or f"sb{self.n_sb}", list(shape), dt))

    def ps(self, shape, dt=F32, name=None):
        self.n_sb += 1
        return self.stack.enter_context(self.nc.psum_tensor(name or f"ps{self.n_sb}", list(shape), dt))

    def op(self, eng, fn, reads=(), writes=(), dma=False, chan=None):
        o = _Op(eng, fn, dma, chan)
        deps = set()
        for k in reads:
            w = self.lastw.get(k)
            if w is not None:
                deps.add(w)
        for k in writes:
            w = self.lastw.get(k)
            if w is not None:
                deps.add(w)
            for r in self.rds.get(k, ()):
                deps.add(r)
        o.idx = len(self.ops[eng])
        self.ops[eng].append(o)
        me = (eng, o.idx)
        deps.discard(me)
        o.deps = list(deps)
        if dma:
            assert chan is not None
            c = self.chan_cnt.get(chan, 0) + 1
            self.chan_cnt[chan] = c
            o.cnt = c
        for k in reads:
            self.rds.setdefault(k, []).append(me)
        for k in writes:
            self.lastw[k] = me
            self.rds[k] = []
        return o

    def dma(self, eng, out, in_, reads=(), writes=(), chan=None, **kw):
        if chan is None:
            chan = ("ch",) + tuple(writes) + tuple(reads)
        return self.op(eng, lambda e: e.dma_start(out=out, in_=in_, **kw), reads, writes, dma=True, chan=chan)

    def emit(self, final_waits=True):
        nc = self.nc
        ops = self.ops
        for e in ENGS:
            for o in ops[e]:
                for (de, di) in o.deps:
                    d = ops[de][di]
                    if d.dma:
                        continue
                    if de == "pe" and e == "pe":
                        continue
                    d.inc = True
        for e in ENGS:
            c = 0
            for o in ops[e]:
                if o.dma:
                    continue
                if o.inc:
                    c += 1
                    o.cnt = c
        st = self.stack
        esem = {e: st.enter_context(nc.semaphore(f"s_{e}")) for e in ENGS}
        csem = {}
        for ch in self.chan_cnt:
            csem[ch] = st.enter_context(nc.semaphore(f"c{len(csem)}"))
        engobj = {"pe": "tensor", "act": "scalar", "dve": "vector", "pool": "gpsimd", "sp": "sync"}
        self.n_waits = 0
        with nc.Block() as block:
            def run(ename):
                def body(eng):
                    waited = {}
                    for o in ops[ename]:
                        need = {}
                        for (de, di) in o.deps:
                            d = ops[de][di]
                            if d.dma:
                                key = ("c", d.chan)
                                val = 16 * d.cnt
                                sem = csem[d.chan]
                            else:
                                if de == "pe" and ename == "pe":
                                    continue
                                key = ("e", de)
                                val = d.cnt
                                sem = esem[de]
                            if val > need.get(key, (0, None))[0]:
                                need[key] = (val, sem)
                        for key, (val, sem) in need.items():
                            if waited.get(key, 0) >= val:
                                continue
                            waited[key] = val
                            eng.wait_ge(sem, val)
                            self.n_waits += 1
                        ins = o.fn(eng)
                        if o.dma:
                            ins.then_inc(csem[o.chan], 16)
                        elif o.inc:
                            ins.then_inc(esem[ename], 1)
                    if final_waits and ename == "sp":
                        for ch, c in self.chan_cnt.items():
                            eng.wait_ge(csem[ch], 16 * c)
                return body
            for e in ENGS:
                getattr(block, engobj[e])(run(e))
        self.stack.close()


T = 256
NCH = 4
C = 64
D = 2048
NCOL = 1442
NPV = 36
NM = 10
NCM = 6
EPS = 1e-6

GROUPS = [("lx", 0, 128, 3), ("lg", 128, 128, 0), ("q", 256, 128, 3), ("k", 384, 128, 3), ("v", 512, 128, 3),
          ("z", 640, 128, 0), ("r", 768, 128, 1), ("rk", 896, 128, 1), ("rv", 1024, 128, 1), ("wa", 1152, 128, 1),
          ("g1", 1280, 128, 1), ("g2", 1408, 32, 1), ("ba", 1440, 2, 0)]


class Bld:
    def __init__(self, g):
        self.g = g

    @staticmethod
    def k(ap):
        return ap.tensor.name

    def mm(self, out, lhsT, rhs, start=True, stop=True):
        self.g.op("pe", lambda e: e.matmul(out, lhsT, rhs, start=start, stop=stop),
                  reads=[self.k(lhsT), self.k(rhs)], writes=[self.k(out)])

    def tr(self, out, in_, ident):
        self.g.op("pe", lambda e: e.transpose(out, in_, ident), reads=[self.k(in_), self.k(ident)], writes=[self.k(out)])

    def act(self, out, in_, func, bias=None, scale=None, eng="act"):
        kw = {}
        rd = [self.k(in_)]
        if bias is not None:
            kw["bias"] = bias
            if not isinstance(bias, float):
                rd.append(self.k(bias))
        if scale is not None:
            kw["scale"] = scale
            if not isinstance(scale, float):
                rd.append(self.k(scale))
        self.g.op("act", lambda e: e.activation(out, in_, func, **kw), reads=rd, writes=[self.k(out)])

    def ts(self, out, in0, s1, s2, op0, op1=None, eng="dve"):
        rd = [self.k(in0)]
        for s in (s1, s2):
            if s is not None and not isinstance(s, float):
                rd.append(self.k(s))
        if op1 is None:
            fn = lambda e: e.tensor_scalar(out, in0, s1, None, op0)
        else:
            fn = lambda e: e.tensor_scalar(out, in0, s1, s2, op0, op1)
        self.g.op(eng, fn, reads=rd, writes=[self.k(out)])

    def tt(self, out, a, b, op, eng="dve"):
        self.g.op(eng, lambda e: e.tensor_tensor(out, a, b, op), reads=[self.k(a), self.k(b)], writes=[self.k(out)])

    def stt(self, out, in0, sc, in1, op0, op1):
        rd = [self.k(in0), self.k(in1)]
        if not isinstance(sc, float):
            rd.append(self.k(sc))
        self.g.op("dve", lambda e: e.scalar_tensor_tensor(out, in0, sc, in1, op0, op1), reads=rd, writes=[self.k(out)])

    def scan(self, out, d0, d1, init):
        rd = [self.k(d0), self.k(d1)]
        if not isinstance(init, float):
            rd.append(self.k(init))
        self.g.op("dve", lambda e: e.tensor_tensor_scan(out, d0, d1, init, ALU.mult, ALU.add), reads=rd, writes=[self.k(out)])

    def cp(self, out, in_, eng="dve"):
        if eng == "act":
            self.g.op("act", lambda e: e.copy(out, in_), reads=[self.k(in_)], writes=[self.k(out)])
        else:
            self.g.op(eng, lambda e: e.tensor_copy(out, in_), reads=[self.k(in_)], writes=[self.k(out)])

    def recip(self, out, in_):
        self.g.op("dve", lambda e: e.reciprocal(out, in_), reads=[self.k(in_)], writes=[self.k(out)])

    def memset(self, ap, v, eng="pool"):
        self.g.op(eng, lambda e: e.memset(ap, v), writes=[self.k(ap)])


def build_LA(S):
    NT = S // T
    nc = bass.Bass("TRN2", target_bir_lowering=False)
    hT = nc.dram_tensor("hT", [D, S], BF16, kind="ExternalInput").ap()
    wc = nc.dram_tensor("wc", [D, NCOL], F32, kind="ExternalInput").ap()
    pvd = nc.dram_tensor("pvd", [128, NPV], F32, kind="ExternalInput").ap()
    matsd = nc.dram_tensor("matsd", [128, NM, 128], F32, kind="ExternalInput").ap()
    cmd = nc.dram_tensor("cmd", [128, NCM, 512], F32, kind="ExternalInput").ap()
    yd = nc.dram_tensor("y", [3, 128, S], BF16, kind="ExternalOutput").ap()
    g = G(nc)
    b = Bld(g)
    wcb = g.sb([128, 16, NCOL], BF16, "wcb")
    pv = g.sb([128, NPV], F32, "pv")
    dv = g.sb([128, 8], F32, "dv")
    mats = g.sb([128, NM, 128], F32, "mats")
    cm = g.sb([128, NCM, 512], F32, "cm")
    hTt = [g.sb([128, 16, T], BF16, f"hTt{i}") for i in range(2)]
    P = {}
    for (nm, c0, rows, H) in GROUPS:
        P[nm] = [g.sb([128, H + T], F32, f"P_{nm}{i}") for i in range(2)]
    PJ = [g.ps([128, 512], F32, f"PJ{i}") for i in range(2)]
    M = [g.ps([128, 512], F32, f"M{i}") for i in range(6)]
    A = [g.sb([128, T], F32, f"A{i}") for i in range(22)]
    Bt = [g.sb([128, 512], F32, f"B{i}") for i in range(16)]
    HS = [g.sb([128, T], F32, f"HS{i}") for i in range(2)]
    YO = [[g.sb([128, T], BF16, f"Y{br}{i}") for i in range(2)] for br in range(3)]
    Sg = g.sb([128, 128], F32, "Sg")
    Sr = g.sb([128, 128], F32, "Sr")
    Usb = g.sb([64, 128], F32, "Usb")
    gcolT = g.sb([64, 8], F32, "gcolT")
    ident = mats[:, 5, :]
    ones = mats[:, 6, :]
    blk64 = mats[:, 7, :]
    RST, MSU, MSL, MUI, NMUI, IDR = range(6)

    def pcol(i):
        return pv[:, i:i + 1]

    for kq in range(4):
        g.dma("pool", wcb[:, 4 * kq:4 * kq + 4, :], wc.rearrange("(k p) n -> p k n", p=128)[:, 4 * kq:4 * kq + 4, :],
              writes=["wcb"], chan="wcb")
    g.dma("sp", pv[:], pvd, writes=["pv"], chan="pv")
    g.dma("sp", mats[:], matsd, writes=["mats"], chan="mats")
    g.dma("sp", cm[:], cmd, writes=["cm"], chan="cm")
    for nm in P:
        for i in range(2):
            b.memset(P[nm][i][:], 0.0)
    b.memset(Sg[:], 0.0)
    b.memset(Sr[:], 0.0)
    b.memset(HS[1][:], 0.0)
    b.act(dv[:, 0:1], pcol(7), AF.Exp, scale=-1.0)
    b.act(dv[:, 0:1], dv[:, 0:1], AF.Ln, bias=1.0)
    b.ts(dv[:, 0:1], dv[:, 0:1], -8.0, None, ALU.mult)
    b.act(dv[:, 1:2], pcol(20), AF.Exp)
    b.ts(dv[:, 1:2], dv[:, 1:2], -1.0, None, ALU.mult)
    b.ts(dv[:, 2:3], pcol(32), -1.0, 1.0, ALU.mult, ALU.add)

    hTv = hT.rearrange("(k p) s -> p k s", p=128)

    def load_h(t):
        g.dma("sp", hTt[t % 2][:], hTv[:, :, t * T:(t + 1) * T], writes=[f"hTt{t % 2}"], chan=f"hTt{t % 2}")

    def proj_groups(t):
        par = t % 2
        fns = []
        for gi, (nm, c0, rows, H) in enumerate(GROUPS):
            def f(gi=gi, nm=nm, c0=c0, rows=rows, H=H):
                pj = PJ[gi % 2]
                for kk in range(16):
                    b.mm(pj[0:rows, 0:T], wcb[:, kk, c0:c0 + rows], hTt[par][:, kk, :], start=(kk == 0), stop=(kk == 15))
                dst = P[nm][par]
                b.cp(dst[0:rows, H:H + T], pj[0:rows, 0:T], eng="act")
                if H > 0 and t > 0:
                    b.cp(dst[0:rows, 0:H], P[nm][1 - par][0:rows, T:T + H], eng="pool")
            fns.append(f)
        return fns

    pending = []

    def fill(n):
        for _ in range(n):
            if pending:
                pending.pop(0)()

    def conv4(out, src, w0col, bias=None):
        if bias is None:
            b.ts(out, src[:, 0:T], pcol(w0col), None, ALU.mult)
        else:
            b.ts(out, src[:, 0:T], pcol(w0col), bias, ALU.mult, ALU.add)
        for j in range(1, 4):
            b.stt(out, src[:, j:j + T], pcol(w0col + j), out, ALU.mult, ALU.add)

    def inverse(N_, M_, R_, Pn, PTn, ncols, hs, psA, psB, psC):
        nb = ncols // 64
        b.tt(R_[0:64, 0:ncols], cm[0:64, IDR, 0:ncols], N_[0:64, 0:ncols], ALU.subtract)
        Pc, PTc = N_, M_
        for lev in range(1, 6):
            last = (lev == 5)
            Pnew, PTnew = (Pn, PTn) if lev % 2 == 1 else (N_, M_)
            for i in range(nb):
                sl = slice(i * 64, (i + 1) * 64)
                if not last:
                    b.mm(psA[0:64, sl], PTc[0:64, sl], Pc[0:64, sl])
                b.mm(psB[0:64, sl], Pc[0:64, sl], PTc[0:64, sl])
            if not last:
                b.cp(Pnew[0:64, 0:ncols], psA[0:64, 0:ncols], eng="act")
            b.cp(PTnew[0:64, 0:ncols], psB[0:64, 0:ncols], eng="dve")
            for i in range(nb):
                sl = slice(i * 64, (i + 1) * 64)
                b.mm(psC[0:64, sl], PTnew[0:64, sl], R_[0:64, sl])
            b.tt(R_[0:64, 0:ncols], R_[0:64, 0:ncols], psC[0:64, 0:ncols], ALU.add)
            Pc, PTc = Pnew, PTnew
            fill(1)

    def lru(t):
        par = t % 2
        lx = P["lx"][par]
        lg = P["lg"][par]
        xc, r_, i_, m_ = A[0], A[1], A[2], A[3]
        conv4(xc[:], lx, 0, bias=pcol(4))
        b.mm(M[0][:, 0:T], mats[:, 0, :], xc[:])
        b.mm(M[1][:, 0:T], mats[:, 1, :], xc[:])
        b.act(r_[:], M[0][:, 0:T], AF.Sigmoid, bias=pcol(5))
        b.act(i_[:], M[1][:, 0:T], AF.Sigmoid, bias=pcol(6))
        b.act(r_[:], r_[:], AF.Exp, scale=dv[:, 0:1])
        b.act(m_[:], r_[:], AF.Square)
        b.act(m_[:], m_[:], AF.Sqrt, bias=1.0, scale=-1.0)
        b.tt(i_[:], i_[:], xc[:], ALU.mult)
        b.tt(i_[:], i_[:], m_[:], ALU.mult)
        b.scan(HS[par][:], r_[:], i_[:], HS[1 - par][:, T - 1:T])
        gsc = A[0]
        b.act(gsc[:], lg[:, 0:T], AF.Square)
        b.ts(gsc[:], gsc[:], 0.044715, 1.0, ALU.mult, ALU.add)
        b.tt(gsc[:], gsc[:], lg[:, 0:T], ALU.mult)
        b.act(gsc[:], gsc[:], AF.Sigmoid, scale=1.5957691216057308)
        b.tt(gsc[:], gsc[:], lg[:, 0:T], ALU.mult)
        b.tt(YO[0][par][:], gsc[:], HS[par][:], ALU.mult)
        g.dma("sp", yd[0, :, t * T:(t + 1) * T], YO[0][par][:], reads=[f"Y0{par}"], chan=f"yo0{par}")

    def gdn(t):
        par = t % 2
        q_, k_, v_ = A[4], A[5], A[6]
        tmp, beta_b, gb, gc_b, egc, kd, Kbeta, Kbe, qd = A[7], A[8], A[9], A[10], A[11], A[12], A[13], A[14], A[15]
        conv4(q_[:], P["q"][par], 8)
        conv4(k_[:], P["k"][par], 12)
        conv4(v_[:], P["v"][par], 16)
        for x_ in (q_, k_, v_):
            b.act(x_[:], x_[:], AF.Silu)
        for x_, sc in ((q_, 128.0 ** -0.5), (k_, 1.0)):
            b.act(tmp[:], x_[:], AF.Square)
            b.mm(M[0][:, 0:T], ones, tmp[:])
            b.act(tmp[:], M[0][:, 0:T], AF.Sqrt, bias=EPS)
            b.recip(tmp[:], tmp[:])
            b.stt(x_[:], x_[:], sc, tmp[:], ALU.mult, ALU.mult)
        ba = P["ba"][par]
        b.mm(M[0][:, 0:T], mats[0:2, 8, :], ba[0:2, 0:T])
        b.mm(M[1][:, 0:T], mats[0:2, 9, :], ba[0:2, 0:T])
        b.act(beta_b[:], M[0][:, 0:T], AF.Sigmoid)
        b.act(gb[:], M[1][:, 0:T], AF.Exp, bias=pcol(21))
        b.act(gb[:], gb[:], AF.Ln, bias=1.0)
        b.ts(gb[:], gb[:], dv[:, 1:2], None, ALU.mult)
        b.scan(gc_b[:], cm[:, RST, 0:T], gb[:], 0.0)
        for n in range(NCH):
            sl = slice(n * C, (n + 1) * C)
            b.ts(kd[:, sl], gc_b[:, sl], gc_b[:, n * C + C - 1:n * C + C], None, ALU.subtract)
        b.act(kd[:], kd[:], AF.Exp, scale=-1.0)
        b.act(egc[:], gc_b[:], AF.Exp)
        b.tt(kd[:], kd[:], k_[:], ALU.mult)
        b.tt(Kbeta[:], k_[:], beta_b[:], ALU.mult)
        b.tt(Kbe[:], Kbeta[:], egc[:], ALU.mult)
        b.tt(v_[:], v_[:], beta_b[:], ALU.mult)
        b.tt(qd[:], q_[:], egc[:], ALU.mult)
        for n in range(NCH):
            b.tr(M[2][0:64, n * 32:(n + 1) * 32], gc_b[0:32, n * C:(n + 1) * C], ident[0:32, 0:32])
        b.cp(gcolT[:, 0:NCH], M[2][0:64, 0:NCH * 32:32])
        DT_, D_, DTsu, R_, Pn, PTn = Bt[0], Bt[1], Bt[2], Bt[3], Bt[4], Bt[5]
        for n in range(NCH):
            sl = slice(n * C, (n + 1) * C)
            b.ts(DT_[0:64, sl], gc_b[0:64, sl], gcolT[:, n:n + 1], 0.0, ALU.subtract, ALU.min)
            b.ts(D_[0:64, sl], gc_b[0:64, sl], gcolT[:, n:n + 1], 0.0, ALU.subtract, ALU.max)
        b.act(DT_[0:64, 0:T], DT_[0:64, 0:T], AF.Exp)
        b.act(D_[0:64, 0:T], D_[0:64, 0:T], AF.Exp, scale=-1.0)
        b.tt(DTsu[0:64, 0:T], DT_[0:64, 0:T], cm[0:64, MSU, 0:T], ALU.mult)
        b.tt(D_[0:64, 0:T], D_[0:64, 0:T], cm[0:64, MSL, 0:T], ALU.mult)
        b.tt(DT_[0:64, 0:T], DT_[0:64, 0:T], cm[0:64, MUI, 0:T], ALU.mult)
        for n in range(NCH):
            sl = slice(n * C, (n + 1) * C)
            b.mm(M[0][0:64, sl], k_[:, sl], Kbeta[:, sl])
            b.mm(M[1][0:64, sl], Kbeta[:, sl], k_[:, sl])
            b.mm(M[2][0:64, sl], k_[:, sl], q_[:, sl])
        b.tt(DTsu[0:64, 0:T], DTsu[0:64, 0:T], M[0][0:64, 0:T], ALU.mult)
        b.tt(D_[0:64, 0:T], D_[0:64, 0:T], M[1][0:64, 0:T], ALU.mult)
        b.tt(DT_[0:64, 0:T], DT_[0:64, 0:T], M[2][0:64, 0:T], ALU.mult)
        inverse(DTsu, D_, R_, Pn, PTn, T, None, M[0], M[1], M[2])
        Kbe_tm, Vb_tm, Kd_tm, U0, WkT = Bt[6], Bt[7], Bt[8], Bt[9], A[13]
        for n in range(NCH):
            sl = slice(n * C, (n + 1) * C)
            b.tr(M[3][0:64, n * 128:(n + 1) * 128], Kbe[:, sl], ident)
            b.tr(M[4][0:64, n * 128:(n + 1) * 128], v_[:, sl], ident)
            b.tr(M[5][0:64, n * 128:(n + 1) * 128], kd[:, sl], ident)
        b.cp(Kbe_tm[0:64, :], M[3][0:64, :], eng="act")
        b.cp(Vb_tm[0:64, :], M[4][0:64, :], eng="dve")
        b.cp(Kd_tm[0:64, :], M[5][0:64, :], eng="act")
        for n in range(NCH):
            sl = slice(n * C, (n + 1) * C)
            b.mm(M[0][0:64, n * 128:(n + 1) * 128], R_[0:64, sl], Vb_tm[0:64, n * 128:(n + 1) * 128])
            b.mm(M[1][:, sl], Kbe_tm[0:64, n * 128:(n + 1) * 128], R_[0:64, sl])
        b.cp(U0[0:64, :], M[0][0:64, :], eng="dve")
        b.cp(WkT[:], M[1][:, 0:T], eng="act")
        for n in range(NCH):
            sl = slice(n * C, (n + 1) * C)
            b.mm(M[2][0:64, 0:128], WkT[:, sl], Sg[:, :])
            b.tt(Usb[:, :], U0[0:64, n * 128:(n + 1) * 128], M[2][0:64, 0:128], ALU.subtract)
            b.mm(M[3][:, sl], Sg[:, :], qd[:, sl], start=True, stop=False)
            b.mm(M[3][:, sl], Usb[:, :], DT_[0:64, sl], start=False, stop=True)
            b.mm(M[4][:, 0:128], Kd_tm[0:64, n * 128:(n + 1) * 128], Usb[:, :])
            b.stt(Sg[:, :], Sg[:, :], egc[:, n * C + C - 1:n * C + C], M[4][:, 0:128], ALU.mult, ALU.add)
            fill(1)
        osb, sz = A[7], A[8]
        b.act(tmp[:], M[3][:, 0:T], AF.Square)
        b.mm(M[0][:, 0:T], ones, tmp[:])
        b.act(tmp[:], M[0][:, 0:T], AF.Sqrt, bias=EPS, scale=1.0 / 128)
        b.recip(tmp[:], tmp[:])
        b.tt(tmp[:], tmp[:], M[3][:, 0:T], ALU.mult)
        b.act(sz[:], P["z"][par][:, 0:T], AF.Silu)
        b.stt(YO[1][par][:], tmp[:], pcol(22), sz[:], ALU.mult, ALU.mult)
        g.dma("sp", yd[1, :, t * T:(t + 1) * T], YO[1][par][:], reads=[f"Y1{par}"], chan=f"yo1{par}")

    def rwkv(t):
        par = t % 2
        r_, k_, v_, wa_, g1_, g2_ = A[0], A[1], A[2], A[3], A[4], A[5]
        tmp = A[6]
        for dst, nm, mucol, rows in ((r_, "r", 23, 128), (k_, "rk", 24, 128), (v_, "rv", 25, 128), (wa_, "wa", 26, 128),
                                     (g1_, "g1", 27, 128), (g2_, "g2", 28, 32)):
            src = P[nm][par]
            b.tt(tmp[0:rows, :], src[0:rows, 0:T], src[0:rows, 1:T + 1], ALU.subtract)
            b.stt(dst[0:rows, :], tmp[0:rows, :], pv[0:rows, mucol:mucol + 1], src[0:rows, 1:T + 1], ALU.mult, ALU.add)
        ld, a_, g_, kk, p_, cum = A[7], A[8], A[9], A[10], A[11], A[12]
        b.act(tmp[0:64, :], wa_[0:64, :], AF.Tanh)
        b.mm(M[0][:, 0:T], mats[0:64, 2, :], tmp[0:64, :])
        b.act(ld[:], M[0][:, 0:T], AF.Sigmoid, bias=pcol(29))
        b.ts(ld[:], ld[:], -0.6065306597126334, None, ALU.mult)
        b.mm(M[1][:, 0:T], mats[64:128, 2, :], wa_[64:128, :])
        b.act(a_[:], M[1][:, 0:T], AF.Sigmoid, bias=pcol(30))
        b.act(g1_[:], g1_[:], AF.Sigmoid)
        b.act(g2_[0:32, :], g2_[0:32, :], AF.Sigmoid)
        b.mm(M[2][:, 0:T], mats[:, 3, :], g1_[:], start=True, stop=False)
        b.mm(M[2][:, 0:T], mats[0:32, 4, :], g2_[0:32, :], start=False, stop=True)
        b.cp(g_[:], M[2][:, 0:T], eng="act")
        b.ts(kk[:], k_[:], pcol(31), None, ALU.mult)
        b.act(tmp[:], kk[:], AF.Square)
        b.mm(M[0][:, 0:T], blk64, tmp[:])
        b.act(tmp[:], M[0][:, 0:T], AF.Sqrt, bias=EPS)
        b.recip(tmp[:], tmp[:])
        b.tt(kk[:], kk[:], tmp[:], ALU.mult)
        b.ts(tmp[:], a_[:], pcol(32), dv[:, 2:3], ALU.mult, ALU.add)
        b.tt(k_[:], k_[:], tmp[:], ALU.mult)
        b.tt(p_[:], kk[:], a_[:], ALU.mult)
        rkb = A[13]
        b.stt(rkb[:], r_[:], pcol(33), k_[:], ALU.mult, ALU.mult)
        b.scan(cum[:], cm[:, RST, 0:T], ld[:], 0.0)
        ecum, encum, ecx, ecl = A[14], A[15], A[16], A[17]
        b.act(ecum[:], cum[:], AF.Exp)
        b.act(encum[:], cum[:], AF.Exp, scale=-1.0)
        b.tt(ecx[:], cum[:], ld[:], ALU.subtract)
        b.act(ecx[:], ecx[:], AF.Exp)
        for n in range(NCH):
            sl = slice(n * C, (n + 1) * C)
            b.ts(ecl[:, sl], cum[:, sl], cum[:, n * C + C - 1:n * C + C], None, ALU.subtract)
        b.act(ecl[:], ecl[:], AF.Exp, scale=-1.0)
        Rh, Kt, Pt, KKh, Kbar, Pbar = A[18], A[19], A[20], A[21], A[6], A[7]
        b.tt(Rh[:], r_[:], ecum[:], ALU.mult)
        b.tt(Kt[:], k_[:], encum[:], ALU.mult)
        b.tt(Pt[:], p_[:], encum[:], ALU.mult)
        b.tt(KKh[:], kk[:], ecx[:], ALU.mult)
        b.tt(Kbar[:], k_[:], ecl[:], ALU.mult)
        b.tt(Pbar[:], p_[:], ecl[:], ALU.mult)
        N_, M_, AkvT, ArkT, nArpT, R_, Pn, PTn = Bt[0], Bt[1], Bt[2], Bt[3], Bt[4], Bt[5], Bt[10], Bt[11]

        def blk(h, n):
            return slice((h * NCH + n) * 64, (h * NCH + n + 1) * 64)

        specs = [(N_, Pt, KKh, MSU), (M_, KKh, Pt, MSL), (AkvT, Kt, KKh, MSU), (ArkT, Kt, Rh, MUI), (nArpT, Pt, Rh, NMUI)]
        for si, (dst, lt, rt, msk) in enumerate(specs):
            for h in range(2):
                hp = slice(64 * h, 64 * h + 64)
                psb = M[(2 * si + h) % 6]
                for n in range(NCH):
                    sl = slice(n * C, (n + 1) * C)
                    b.mm(psb[0:64, sl], lt[hp, sl], rt[hp, sl])
                b.tt(dst[0:64, h * T:(h + 1) * T], psb[0:64, 0:T], cm[0:64, msk, 0:T], ALU.mult)
        inverse(N_, M_, R_, Pn, PTn, 512, None, M[0], M[1], M[2])
        V_tm, KK_tm, Kb_tm, nPb_tm = Bt[6], Bt[7], Bt[8], Bt[9]
        for n in range(NCH):
            sl = slice(n * C, (n + 1) * C)
            b.tr(M[0][0:64, n * 128:(n + 1) * 128], v_[:, sl], ident)
            b.tr(M[1][0:64, n * 128:(n + 1) * 128], KKh[:, sl], ident)
            b.tr(M[2][0:64, n * 128:(n + 1) * 128], Kbar[:, sl], ident)
            b.tr(M[3][0:64, n * 128:(n + 1) * 128], Pbar[:, sl], ident)
        b.cp(V_tm[0:64, :], M[0][0:64, :], eng="act")
        b.cp(KK_tm[0:64, :], M[1][0:64, :], eng="dve")
        b.cp(Kb_tm[0:64, :], M[2][0:64, :], eng="act")
        b.ts(nPb_tm[0:64, :], M[3][0:64, :], -1.0, None, ALU.mult)
        Y_, U0, W1T = Bt[12], Bt[13], A[8]

        def tm(n, h):
            return slice(n * 128 + h * 64, n * 128 + h * 64 + 64)

        for n in range(NCH):
            for h in range(2):
                b.mm(M[4][0:64, tm(n, h)], AkvT[0:64, blk(h, n)], V_tm[0:64, tm(n, h)])
        b.cp(Y_[0:64, :], M[4][0:64, :], eng="dve")
        for n in range(NCH):
            for h in range(2):
                b.mm(M[5][0:64, tm(n, h)], R_[0:64, blk(h, n)], Y_[0:64, tm(n, h)])
                b.mm(M[0][64 * h:64 * h + 64, n * C:(n + 1) * C], KK_tm[0:64, tm(n, h)], R_[0:64, blk(h, n)])
        b.cp(U0[0:64, :], M[5][0:64, :], eng="dve")
        b.cp(W1T[:], M[0][:, 0:T], eng="act")
        for n in range(NCH):
            sl = slice(n * C, (n + 1) * C)
            b.mm(M[1][0:64, 0:128], W1T[:, sl], Sr[:, :])
            b.tt(Usb[:, :], U0[0:64, n * 128:(n + 1) * 128], M[1][0:64, 0:128], ALU.add)
            b.mm(M[2][:, sl], Sr[:, :], Rh[:, sl], start=True, stop=False)
            for h in range(2):
                hp = slice(64 * h, 64 * h + 64)
                last = (h == 1)
                b.mm(M[2][hp, sl], V_tm[0:64, tm(n, h)], ArkT[0:64, blk(h, n)], start=False, stop=False)
                b.mm(M[2][hp, sl], Usb[:, 64 * h:64 * h + 64], nArpT[0:64, blk(h, n)], start=False, stop=True)
                b.mm(M[3][hp, 64 * h:64 * h + 64], Kb_tm[0:64, tm(n, h)], V_tm[0:64, tm(n, h)], start=True, stop=False)
                b.mm(M[3][hp, 64 * h:64 * h + 64], nPb_tm[0:64, tm(n, h)], Usb[:, 64 * h:64 * h + 64], start=False, stop=True)
            for h in range(2):
                hp = slice(64 * h, 64 * h + 64)
                hc = slice(64 * h, 64 * h + 64)
                b.stt(Sr[hp, hc], Sr[hp, hc], ecum[hp, n * C + C - 1:n * C + C], M[3][hp, hc], ALU.mult, ALU.add)
            fill(1)
        osb, cen = A[9 + 1], A[11]
        b.cp(osb[:], M[2][:, 0:T], eng="act")
        b.mm(M[4][:, 0:T], blk64, osb[:])
        b.stt(cen[:], M[4][:, 0:T], -1.0 / 64, osb[:], ALU.mult, ALU.add)
        b.act(tmp[:], cen[:], AF.Square)
        b.mm(M[5][:, 0:T], blk64, tmp[:])
        b.act(tmp[:], M[5][:, 0:T], AF.Sqrt, bias=64e-5, scale=1.0 / 64)
        b.recip(tmp[:], tmp[:])
        b.tt(cen[:], cen[:], tmp[:], ALU.mult)
        b.ts(cen[:], cen[:], pcol(34), pcol(35), ALU.mult, ALU.add)
        b.mm(M[4][:, 0:T], blk64, rkb[:])
        b.tt(tmp[:], M[4][:, 0:T], v_[:], ALU.mult)
        b.tt(cen[:], cen[:], tmp[:], ALU.add)
        b.tt(YO[2][par][:], cen[:], g_[:], ALU.mult)
        g.dma("sp", yd[2, :, t * T:(t + 1) * T], YO[2][par][:], reads=[f"Y2{par}"], chan=f"yo2{par}")

    branches = "abc"
    load_h(0)
    for f in proj_groups(0):
        f()
    for t in range(NT):
        if t + 1 < NT:
            load_h(t + 1)
            pending.extend(proj_groups(t + 1))
        if "a" in branches:
            lru(t)
        if "b" in branches:
            gdn(t)
        if "c" in branches:
            rwkv(t)
        fill(len(pending))
    g.emit()
    return nc


def la_cols(c):
    cols = []
    for base in (0, 1024, 2048, 3072, 4096, 5120, 6160, 7184, 8208):
        cols += list(range(base + c * 128, base + c * 128 + 128))
    cols += list(range(9232, 9360))
    cols += list(range(9360, 9520))
    cols += [6144 + c, 6152 + c]
    return np.array(cols)


def la_consts():
    cmv = np.zeros((128, NCM, 512), np.float32)
    j = np.arange(64)[:, None]
    i = np.arange(512)[None, :] % 64
    cmv[:, 0, :] = 1.0
    cmv[:, 0, ::64] = 0.0
    cmv[0:64, 1, :] = (j < i)
    cmv[0:64, 2, :] = (j > i)
    cmv[0:64, 3, :] = (j <= i)
    cmv[0:64, 4, :] = -1.0 * (j <= i)
    cmv[0:64, 5, :] = (j == i)
    return cmv


def la_inputs(inp, l, c):
    f = np.float32
    sl = slice(c * 128, (c + 1) * 128)
    pv = np.zeros((128, NPV), f)
    pv[:, 0:4] = inp["lru_conv_w"][l][:, sl].T
    pv[:, 4] = inp["lru_conv_b"][l][sl]
    pv[:, 5] = inp["lru_b_a"][l][sl]
    pv[:, 6] = inp["lru_b_x"][l][sl]
    pv[:, 7] = inp["lru_lambda"][l][sl]
    gw = inp["gdn_conv_w"][l]
    pv[:, 8:12] = gw[:, c * 128:(c + 1) * 128].T
    pv[:, 12:16] = gw[:, 1024 + c * 128:1024 + (c + 1) * 128].T
    pv[:, 16:20] = gw[:, 2048 + c * 128:2048 + (c + 1) * 128].T
    pv[:, 20] = inp["gdn_a_log"][l][c]
    pv[:, 21] = inp["gdn_dt_bias"][l][c]
    pv[:, 22] = inp["gdn_norm_w"][l]
    mu = inp["rwkv_mu"][l]
    pv[:, 23] = mu[c * 128:(c + 1) * 128]
    pv[:, 24] = mu[1024 + c * 128:1024 + (c + 1) * 128]
    pv[:, 25] = mu[2048 + c * 128:2048 + (c + 1) * 128]
    pv[:, 26] = mu[3072:3200]
    pv[:, 27] = mu[3200:3328]
    pv[0:32, 28] = mu[3328:3360]
    pv[:, 29] = inp["rwkv_w0"][l][sl]
    pv[:, 30] = inp["rwkv_a0"][l][sl]
    pv[:, 31] = inp["rwkv_k_k"][l][sl]
    pv[:, 32] = inp["rwkv_k_a"][l][sl]
    pv[:, 33] = inp["rwkv_r_k"][l].reshape(-1)[sl]
    pv[:, 34] = inp["rwkv_ln_w"][l][sl]
    pv[:, 35] = inp["rwkv_ln_b"][l][sl]
    mats = np.zeros((128, NM, 128), f)
    for gl in range(2):
        mats[gl * 64:(gl + 1) * 64, 0, gl * 64:(gl + 1) * 64] = inp["lru_w_a"][l][2 * c + gl]
        mats[gl * 64:(gl + 1) * 64, 1, gl * 64:(gl + 1) * 64] = inp["lru_w_x"][l][2 * c + gl]
    mats[0:64, 2, :] = inp["rwkv_w_up"][l][:, sl]
    mats[64:128, 2, :] = inp["rwkv_a_up"][l][:, sl]
    mats[:, 3, :] = inp["rwkv_g_up"][l][0:128, sl]
    mats[0:32, 4, :] = inp["rwkv_g_up"][l][128:160, sl]
    mats[:, 5, :] = np.eye(128, dtype=f)
    mats[:, 6, :] = 1.0
    mats[0:64, 7, 0:64] = 1.0
    mats[64:128, 7, 64:128] = 1.0
    mats[0, 8, :] = 1.0
    mats[1, 9, :] = 1.0
    wc = np.ascontiguousarray(inp["w_in"][l][:, la_cols(c)])
    return dict(wc=wc, pvd=pv, matsd=mats, cmd=la_consts())


TB = 512
DFF = 5632
NF = DFF // 128


def build_LB(NTOK, mode="full"):
    NTL = NTOK // TB
    nc = bass.Bass("TRN2", target_bir_lowering=False)
    xTd = nc.dram_tensor("xT", [D, NTOK], F32, kind="ExternalInput").ap()
    nvd = nc.dram_tensor("nvd", [128, 16, 4], F32, kind="ExternalInput").ap()
    onesd = nc.dram_tensor("onesd", [128, 128], F32, kind="ExternalInput").ap()
    hTo = nc.dram_tensor("hT_out", [D, NTOK], BF16, kind="ExternalOutput").ap()
    full = (mode == "full")
    if full:
        yTd = nc.dram_tensor("yT", [3072, NTOK], BF16, kind="ExternalInput").ap()
        hTd = nc.dram_tensor("hT", [D, NTOK], BF16, kind="ExternalInput").ap()
        wg3 = nc.dram_tensor("wg3", [D, 6144], F32, kind="ExternalInput").ap()
        wbr = nc.dram_tensor("wbr", [3072, D], F32, kind="ExternalInput").ap()
        wo = nc.dram_tensor("wo", [D, D], F32, kind="ExternalInput").ap()
        wfg = nc.dram_tensor("wfg", [D, DFF], F32, kind="ExternalInput").ap()
        wfu = nc.dram_tensor("wfu", [D, DFF], F32, kind="ExternalInput").ap()
        wfd = nc.dram_tensor("wfd", [DFF, D], F32, kind="ExternalInput").ap()
        xTo = nc.dram_tensor("xT_out", [D, NTOK], F32, kind="ExternalOutput").ap()
    g = G(nc)
    b = Bld(g)
    nv = g.sb([128, 16, 4], F32, "nv")
    ones = g.sb([128, 128], F32, "ones")
    xt = g.sb([128, 16, TB], F32, "xt")
    hb = g.sb([128, 16, TB], BF16, "hb")
    rstd = g.sb([128, TB], F32, "rstd")
    sq = [g.sb([128, TB], F32, f"sq{i}") for i in range(2)]
    SS = g.ps([128, 512], F32, "SS")
    g.dma("sp", nv[:], nvd, writes=["nv"], chan="nv")
    g.dma("sp", ones[:], onesd, writes=["ones"], chan="ones")
    if full:
        big = g.sb([128, NF * TB], BF16, "big")
        yt = big[:, 0:24 * TB].rearrange("p (k t) -> p k t", t=TB)
        mixT = big[:, 24 * TB:40 * TB].rearrange("p (k t) -> p k t", t=TB)
        aT = big[:, :].rearrange("p (k t) -> p k t", t=TB)
        oT = g.sb([128, 16, TB], F32, "oT")
        WP = [g.sb([128, 12288], BF16, f"WP{i}") for i in range(2)]
        gsb = [g.sb([128, TB], F32, f"gsb{i}") for i in range(2)]
        tmpb = [g.sb([128, TB], F32, f"tmpb{i}") for i in range(2)]
        macc = g.sb([128, 4, TB], F32, "macc")
        pA = [g.ps([128, 512], F32, f"pA{i}") for i in range(2)]
        pB = [g.ps([128, 512], F32, f"pB{i}") for i in range(2)]

    xTv = xTd.rearrange("(k p) s -> p k s", p=128)
    hTov = hTo.rearrange("(k p) s -> p k s", p=128)

    def norm_stats_finish():
        b.act(rstd[:], SS[:, 0:TB], AF.Sqrt, bias=EPS, scale=1.0 / D)
        b.recip(rstd[:], rstd[:])

    def sumsq(src_fn):
        for k in range(16):
            s_ = sq[k % 2]
            b.act(s_[:], src_fn(k), AF.Square)
            b.mm(SS[:, 0:TB], ones[:, :], s_[:], start=(k == 0), stop=(k == 15))
        norm_stats_finish()

    def residual_add(j):
        for k in range(16):
            t_ = sq[k % 2]
            b.stt(t_[:], oT[:, k, :], nv[:, k, j:j + 1], rstd[:], ALU.mult, ALU.mult)
            b.tt(xt[:, k, :], xt[:, k, :], t_[:], ALU.add)

    def norm_to(dst, j):
        sumsq(lambda k: xt[:, k, :])
        for k in range(16):
            b.stt(dst[:, k, :], xt[:, k, :], nv[:, k, j:j + 1], rstd[:], ALU.mult, ALU.mult)

    items = []

    def wview(buf, off, K, n):
        return buf[:, off:off + K * n].rearrange("p (k n) -> p k n", n=n)

    for tl in range(NTL):
        tsl = slice(tl * TB, (tl + 1) * TB)

        def start_tile(buf, tsl=tsl):
            g.dma("sp", xt[:], xTv[:, :, tsl], writes=["xt"], chan="xt")
            if full:
                g.dma("sp", hb[:], hTd.rearrange("(k p) s -> p k s", p=128)[:, :, tsl], writes=["hb"], chan="hb")
                g.dma("sp", yt, yTd.rearrange("(k p) s -> p k s", p=128)[:, :, tsl], writes=["big"], chan="big")

        if not full:
            def only_norm(buf, tsl=tsl, st=start_tile):
                st(None)
                norm_to(hb, 3)
                g.dma("sp", hTov[:, :, tsl], hb[:], reads=["hb"], chan="hbo")
            items.append((None, only_norm))
            continue

        for dcg in range(4):
            for br in range(3):
                def ld(buf, dcg=dcg, br=br):
                    g.dma("pool", wview(buf, 0, 16, 512), wg3.rearrange("(k p) n -> p k n", p=128)[:, :, br * 2048 + dcg * 512: br * 2048 + dcg * 512 + 512],
                          writes=[buf.name], chan=buf.name)
                    g.dma("pool", wview(buf, 8192, 8, 512), wbr.rearrange("(k p) n -> p k n", p=128)[:, br * 8:br * 8 + 8, dcg * 512:dcg * 512 + 512],
                          writes=[buf.name], chan=buf.name)

                def cmp(buf, dcg=dcg, br=br, first=(dcg == 0 and br == 0), tsl=tsl, st=start_tile):
                    if first:
                        st(None)
                    Wg = wview(buf, 0, 16, 512)
                    Wb = wview(buf, 8192, 8, 512)
                    for dci in range(4):
                        dc = dcg * 4 + dci
                        cs = slice(dci * 128, dci * 128 + 128)
                        pg, pb = pA[dci % 2], pB[dci % 2]
                        for k in range(16):
                            b.mm(pg[:, 0:TB], Wg[:, k, cs], hb[:, k, :], start=(k == 0), stop=(k == 15))
                        b.act(gsb[dci % 2][:], pg[:, 0:TB], AF.Sigmoid)
                        for k in range(8):
                            b.mm(pb[:, 0:TB], Wb[:, k, cs], yt[:, br * 8 + k, :], start=(k == 0), stop=(k == 7))
                        if br == 0:
                            b.tt(macc[:, dci, :], gsb[dci % 2][:], pb[:, 0:TB], ALU.mult)
                        elif br == 1:
                            b.tt(tmpb[dci % 2][:], gsb[dci % 2][:], pb[:, 0:TB], ALU.mult)
                            b.tt(macc[:, dci, :], macc[:, dci, :], tmpb[dci % 2][:], ALU.add)
                        else:
                            b.tt(tmpb[dci % 2][:], gsb[dci % 2][:], pb[:, 0:TB], ALU.mult)
                            b.tt(mixT[:, dc, :], macc[:, dci, :], tmpb[dci % 2][:], ALU.add)
                items.append((ld, cmp))

        def dense_items(w_ap, K, ncols_total, pcols, rhs_fn, evac_fn, post_fn=None):
            npan = ncols_total // pcols
            for pi in range(npan):
                def ld(buf, pi=pi):
                    g.dma("pool", wview(buf, 0, K, pcols), w_ap.rearrange("(k p) n -> p k n", p=128)[:, :, pi * pcols:(pi + 1) * pcols],
                          writes=[buf.name], chan=buf.name)

                def cmp(buf, pi=pi, lastp=(pi == npan - 1)):
                    W = wview(buf, 0, K, pcols)
                    for ci in range(pcols // 128):
                        dc = pi * (pcols // 128) + ci
                        ps_ = pA[dc % 2]
                        for k in range(K):
                            b.mm(ps_[:, 0:TB], W[:, k, ci * 128:(ci + 1) * 128], rhs_fn(k), start=(k == 0), stop=(k == K - 1))
                        evac_fn(dc, ps_)
                    if lastp and post_fn is not None:
                        post_fn()
                items.append((ld, cmp))

        def evac_o(dc, ps_):
            b.cp(oT[:, dc, :], ps_[:, 0:TB], eng="act")
            s_ = sq[dc % 2]
            b.act(s_[:], ps_[:, 0:TB], AF.Square)
            b.mm(SS[:, 0:TB], ones[:, :], s_[:], start=(dc == 0), stop=(dc == 15))

        def post_mix():
            norm_stats_finish()
            residual_add(0)
            norm_to(hb, 1)

        def post_ffn(tsl=tsl):
            norm_stats_finish()
            residual_add(2)
            g.dma("sp", xTo.rearrange("(k p) s -> p k s", p=128)[:, :, tsl], xt[:], reads=["xt"], chan="xto")
            norm_to(hb, 3)
            g.dma("sp", hTov[:, :, tsl], hb[:], reads=["hb"], chan="hbo")

        dense_items(wo, 16, D, 512, lambda k: mixT[:, k, :], evac_o, post_mix)
        for fi in range(DFF // 256):
            def ld(buf, fi=fi):
                g.dma("pool", wview(buf, 0, 16, 256), wfg.rearrange("(k p) n -> p k n", p=128)[:, :, fi * 256:fi * 256 + 256], writes=[buf.name], chan=buf.name)
                g.dma("pool", wview(buf, 4096, 16, 256), wfu.rearrange("(k p) n -> p k n", p=128)[:, :, fi * 256:fi * 256 + 256], writes=[buf.name], chan=buf.name)

            def cmp(buf, fi=fi):
                Wg_ = wview(buf, 0, 16, 256)
                Wu_ = wview(buf, 4096, 16, 256)
                for ci in range(2):
                    fc = fi * 2 + ci
                    pg, pu = pA[ci], pB[ci]
                    for k in range(16):
                        b.mm(pg[:, 0:TB], Wg_[:, k, ci * 128:(ci + 1) * 128], hb[:, k, :], start=(k == 0), stop=(k == 15))
                    for k in range(16):
                        b.mm(pu[:, 0:TB], Wu_[:, k, ci * 128:(ci + 1) * 128], hb[:, k, :], start=(k == 0), stop=(k == 15))
                    b.act(gsb[ci][:], pg[:, 0:TB], AF.Silu)
                    b.tt(aT[:, fc, :], gsb[ci][:], pu[:, 0:TB], ALU.mult)
            items.append((ld, cmp))
        dense_items(wfd, NF, D, 256, lambda k: aT[:, k, :], evac_o, post_ffn)

    nb = 0
    bufs = []
    for it in items:
        bufs.append(None)
    def buf_of(i):
        return WP[i % 2] if full else None
    if items and items[0][0] is not None:
        items[0][0](buf_of(0))
    for i, (ld, cmp) in enumerate(items):
        if i + 1 < len(items) and items[i + 1][0] is not None:
            items[i + 1][0](buf_of(i + 1))
        cmp(buf_of(i))
    g.emit()
    return nc


def lb_nv(inp, l):
    nv = np.zeros((128, 16, 4), np.float32)
    vecs = [inp["norm_mix_post"][l], inp["norm_ffn_pre"][l], inp["norm_ffn_post"][l],
            inp["norm_mix_pre"][l + 1] if l + 1 < inp["norm_mix_pre"].shape[0] else np.ones(D, np.float32)]
    for j, v in enumerate(vecs):
        nv[:, :, j] = v.reshape(16, 128).T
    return nv


def _run(nc, maps):
    return run_bass_kernel_spmd(nc, maps, core_ids=list(range(len(maps)))).results


def kernel(**inputs):
    inp = {k: np.asarray(v) for k, v in inputs.items()}
    f32 = np.float32
    NCORE = 8
    x = inp["x"][0].astype(f32)
    S = x.shape[0]
    NTOK = S // NCORE
    L = inp["w_in"].shape[0]
    ones = np.ones((128, 128), f32)
    xT_sh = [np.ascontiguousarray(x[c * NTOK:(c + 1) * NTOK].T) for c in range(NCORE)]
    nv0 = np.zeros((128, 16, 4), f32)
    nv0[:, :, 3] = inp["norm_mix_pre"][0].reshape(16, 128).T
    nc0 = build_LB(NTOK, "norm")
    r0 = _run(nc0, [dict(xT=xT_sh[c], nvd=nv0, onesd=ones) for c in range(NCORE)])
    hT_sh = [r0[c]["hT_out"] for c in range(NCORE)]
    ncA = build_LA(S)
    ncB = build_LB(NTOK, "full")
    for l in range(L):
        hT_all = np.ascontiguousarray(np.concatenate(hT_sh, axis=1))
        mapsA = []
        for c in range(NCORE):
            m = la_inputs(inp, l, c)
            m["hT"] = hT_all
            mapsA.append(m)
        rA = _run(ncA, mapsA)
        yfull = np.stack([rA[c]["y"] for c in range(NCORE)], axis=1).reshape(3072, S)
        del rA, mapsA
        wg3 = np.ascontiguousarray(inp["w_in"][l][:, 9520:])
        wbr = np.ascontiguousarray(inp["w_branch"][l].reshape(3072, D))
        nvl = lb_nv(inp, l)
        mapsB = [dict(xT=xT_sh[c], yT=np.ascontiguousarray(yfull[:, c * NTOK:(c + 1) * NTOK]), hT=hT_sh[c],
                      nvd=nvl, onesd=ones, wg3=wg3, wbr=wbr, wo=inp["w_out"][l], wfg=inp["ffn_w_gate"][l],
                      wfu=inp["ffn_w_up"][l], wfd=inp["ffn_w_down"][l]) for c in range(NCORE)]
        rB = _run(ncB, mapsB)
        xT_sh = [rB[c]["xT_out"] for c in range(NCORE)]
        hT_sh = [rB[c]["hT_out"] for c in range(NCORE)]
        del rB, mapsB
    out = np.concatenate([np.asarray(xT_sh[c], dtype=f32).T for c in range(NCORE)], axis=0)
    return np.ascontiguousarray(out[None]).astype(f32)
```

```python
import ml_dtypes
import numpy as np
from contextlib import ExitStack
import concourse.bass as bass
import concourse.mybir as mybir
from concourse.bass_utils import run_bass_kernel_spmd

F32 = mybir.dt.float32
BF16 = mybir.dt.bfloat16
ALU = mybir.AluOpType
AF = mybir.ActivationFunctionType

ENGS = ("pe", "act", "dve", "pool", "sp")


class _Op:
    __slots__ = ("eng", "fn", "deps", "dma", "chan", "cnt", "inc", "idx")

    def __init__(self, eng, fn, dma, chan):
        self.eng = eng
        self.fn = fn
        self.deps = []
        self.dma = dma
        self.chan = chan
        self.cnt = 0
        self.inc = False
        self.idx = 0


class G:
    def __init__(self, nc):
        self.nc = nc
        self.ops = {e: [] for e in ENGS}
        self.lastw = {}
        self.rds = {}
        self.chan_cnt = {}
        self.stack = ExitStack()
        self.n_sb = 0

    def sb(self, shape, dt=F32, name=None):
        self.n_sb += 1
        return self.stack.enter_context(self.nc.sbuf_tensor(name or f"sb{self.n_sb}", list(shape), dt))

    def ps(self, shape, dt=F32, name=None):
        self.n_sb += 1
        return self.stack.enter_context(self.nc.psum_tensor(name or f"ps{self.n_sb}", list(shape), dt))

    def op(self, eng, fn, reads=(), writes=(), dma=False, chan=None):
        o = _Op(eng, fn, dma, chan)
        deps = set()
        for k in reads:
            w = self.lastw.get(k)
            if w is not None:
                deps.add(w)
        for k in writes:
            w = self.lastw.get(k)
            if w is not None:
                deps.add(w)
            for r in self.rds.get(k, ()):
                deps.add(r)
        o.idx = len(self.ops[eng])
        self.ops[eng].append(o)
        me = (eng, o.idx)
        deps.discard(me)
        o.deps = list(deps)
        if dma:
            assert chan is not None
            c = self.chan_cnt.get(chan, 0) + 1
            self.chan_cnt[chan] = c
            o.cnt = c
        for k in reads:
            self.rds.setdefault(k, []).append(me)
        for k in writes:
            self.lastw[k] = me
            self.rds[k] = []
        return o

    def dma(self, eng, out, in_, reads=(), writes=(), chan=None, **kw):
        if chan is None:
            chan = ("ch",) + tuple(writes) + tuple(reads)
        return self.op(eng, lambda e: e.dma_start(out=out, in_=in_, **kw), reads, writes, dma=True, chan=chan)

    def emit(self, final_waits=True):
        nc = self.nc
        ops = self.ops
        for e in ENGS:
            for o in ops[e]:
                for (de, di) in o.deps:
                    d = ops[de][di]
                    if d.dma:
                        continue
                    if de == "pe" and e == "pe":
                        continue
                    d.inc = True
        for e in ENGS:
            c = 0
            for o in ops[e]:
                if o.dma:
                    continue
                if o.inc:
                    c += 1
                    o.cnt = c
        st = self.stack
        esem = {e: st.enter_context(nc.semaphore(f"s_{e}")) for e in ENGS}
        csem = {}
        for ch in self.chan_cnt:
            csem[ch] = st.enter_context(nc.semaphore(f"c{len(csem)}"))
        engobj = {"pe": "tensor", "act": "scalar", "dve": "vector", "pool": "gpsimd", "sp": "sync"}
        self.n_waits = 0
        with nc.Block() as block:
            def run(ename):
                def body(eng):
                    waited = {}
                    for o in ops[ename]:
                        need = {}
                        for (de, di) in o.deps:
                            d = ops[de][di]
                            if d.dma:
                                key = ("c", d.chan)
                                val = 16 * d.cnt
                                sem = csem[d.chan]
                            else:
                                if de == "pe" and ename == "pe":
                                    continue
                                key = ("e", de)
                                val = d.cnt
                                sem = esem[de]
                            if val > need.get(key, (0, None))[0]:
                                need[key] = (val, sem)
                        for key, (val, sem) in need.items():
                            if waited.get(key, 0) >= val:
                                continue
                            waited[key] = val
                            eng.wait_ge(sem, val)
                            self.n_waits += 1
                        ins = o.fn(eng)
                        if o.dma:
                            ins.then_inc(csem[o.chan], 16)
                        elif o.inc:
                            ins.then_inc(esem[ename], 1)
                    if final_waits and ename == "sp":
                        for ch, c in self.chan_cnt.items():
                            eng.wait_ge(csem[ch], 16 * c)
                return body
            for e in ENGS:
                getattr(block, engobj[e])(run(e))
        self.stack.close()


T = 256
NCH = 4
C = 64
D = 2048
NCOL = 1442
NPV = 36
NM = 10
NCM = 6
EPS = 1e-6

GROUPS = [("lx", 0, 128, 3), ("lg", 128, 128, 0), ("q", 256, 128, 3), ("k", 384, 128, 3), ("v", 512, 128, 3),
          ("z", 640, 128, 0), ("r", 768, 128, 1), ("rk", 896, 128, 1), ("rv", 1024, 128, 1), ("wa", 1152, 128, 1),
          ("g1", 1280, 128, 1), ("g2", 1408, 32, 1), ("ba", 1440, 2, 0)]


class Bld:
    def __init__(self, g):
        self.g = g

    @staticmethod
    def k(ap):
        return ap.tensor.name

    def mm(self, out, lhsT, rhs, start=True, stop=True):
        self.g.op("pe", lambda e: e.matmul(out, lhsT, rhs, start=start, stop=stop),
                  reads=[self.k(lhsT), self.k(rhs)], writes=[self.k(out)])

    def tr(self, out, in_, ident):
        self.g.op("pe", lambda e: e.transpose(out, in_, ident), reads=[self.k(in_), self.k(ident)], writes=[self.k(out)])

    def act(self, out, in_, func, bias=None, scale=None, eng="act"):
        kw = {}
        rd = [self.k(in_)]
        if bias is not None:
            kw["bias"] = bias
            if not isinstance(bias, float):
                rd.append(self.k(bias))
        if scale is not None:
            kw["scale"] = scale
            if not isinstance(scale, float):
                rd.append(self.k(scale))
        self.g.op("act", lambda e: e.activation(out, in_, func, **kw), reads=rd, writes=[self.k(out)])

    def ts(self, out, in0, s1, s2, op0, op1=None, eng="dve"):
        rd = [self.k(in0)]
        for s in (s1, s2):
            if s is not None and not isinstance(s, float):
                rd.append(self.k(s))
        if op1 is None:
            fn = lambda e: e.tensor_scalar(out, in0, s1, None, op0)
        else:
            fn = lambda e: e.tensor_scalar(out, in0, s1, s2, op0, op1)
        self.g.op(eng, fn, reads=rd, writes=[self.k(out)])

    def tt(self, out, a, b, op, eng="dve"):
        self.g.op(eng, lambda e: e.tensor_tensor(out, a, b, op), reads=[self.k(a), self.k(b)], writes=[self.k(out)])

    def stt(self, out, in0, sc, in1, op0, op1):
        rd = [self.k(in0), self.k(in1)]
        if not isinstance(sc, float):
            rd.append(self.k(sc))
        self.g.op("dve", lambda e: e.scalar_tensor_tensor(out, in0, sc, in1, op0, op1), reads=rd, writes=[self.k(out)])

    def scan(self, out, d0, d1, init):
        rd = [self.k(d0), self.k(d1)]
        if not isinstance(init, float):
            rd.append(self.k(init))
        self.g.op("dve", lambda e: e.tensor_tensor_scan(out, d0, d1, init, ALU.mult, ALU.add), reads=rd, writes=[self.k(out)])

    def cp(self, out, in_, eng="dve"):
        if eng == "act":
            self.g.op("act", lambda e: e.copy(out, in_), reads=[self.k(in_)], writes=[self.k(out)])
        else:
            self.g.op(eng, lambda e: e.tensor_copy(out, in_), reads=[self.k(in_)], writes=[self.k(out)])

    def recip(self, out, in_):
        self.g.op("dve", lambda e: e.reciprocal(out, in_), reads=[self.k(in_)], writes=[self.k(out)])

    def memset(self, ap, v, eng="pool"):
        self.g.op(eng, lambda e: e.memset(ap, v), writes=[self.k(ap)])


def build_LA(S):
    NT = S // T
    nc = bass.Bass("TRN2", target_bir_lowering=False)
    hT = nc.dram_tensor("hT", [D, S], BF16, kind="ExternalInput").ap()
    wc = nc.dram_tensor("wc", [D, NCOL], F32, kind="ExternalInput").ap()
    pvd = nc.dram_tensor("pvd", [128, NPV], F32, kind="ExternalInput").ap()
    matsd = nc.dram_tensor("matsd", [128, NM, 128], F32, kind="ExternalInput").ap()
    cmd = nc.dram_tensor("cmd", [128, NCM, 512], F32, kind="ExternalInput").ap()
    yd = nc.dram_tensor("y", [3, 128, S], BF16, kind="ExternalOutput").ap()
    g = G(nc)
    b = Bld(g)
    wcb = g.sb([128, 16, NCOL], BF16, "wcb")
    pv = g.sb([128, NPV], F32, "pv")
    dv = g.sb([128, 8], F32, "dv")
    mats = g.sb([128, NM, 128], F32, "mats")
    cm = g.sb([128, NCM, 512], F32, "cm")
    hTt = [g.sb([128, 16, T], BF16, f"hTt{i}") for i in range(2)]
    P = {}
    for (nm, c0, rows, H) in GROUPS:
        P[nm] = [g.sb([128, H + T], F32, f"P_{nm}{i}") for i in range(2)]
    PJ = [g.ps([128, 512], F32, f"PJ{i}") for i in range(2)]
    M = [g.ps([128, 512], F32, f"M{i}") for i in range(6)]
    A = [g.sb([128, T], F32, f"A{i}") for i in range(22)]
    Bt = [g.sb([128, 512], F32, f"B{i}") for i in range(16)]
    HS = [g.sb([128, T], F32, f"HS{i}") for i in range(2)]
    YO = [[g.sb([128, T], BF16, f"Y{br}{i}") for i in range(2)] for br in range(3)]
    Sg = g.sb([128, 128], F32, "Sg")
    Sr = g.sb([128, 128], F32, "Sr")
    Usb = g.sb([64, 128], F32, "Usb")
    gcolT = g.sb([64, 8], F32, "gcolT")
    PRa = g.sb([128, 1024], F32, "PRa")
    PRb = g.sb([128, 1024], F32, "PRb")
    ident = mats[:, 5, :]
    ones = mats[:, 6, :]
    blk64 = mats[:, 7, :]
    RST, MSU, MSL, MUI, NMUI, IDR = range(6)

    def pcol(i):
        return pv[:, i:i + 1]

    for kq in range(4):
        g.dma("pool", wcb[:, 4 * kq:4 * kq + 4, :], wc.rearrange("(k p) n -> p k n", p=128)[:, 4 * kq:4 * kq + 4, :],
              writes=["wcb"], chan="wcb")
    g.dma("sp", pv[:], pvd, writes=["pv"], chan="pv")
    g.dma("sp", mats[:], matsd, writes=["mats"], chan="mats")
    g.dma("sp", cm[:], cmd, writes=["cm"], chan="cm")
    for nm in P:
        for i in range(2):
            b.memset(P[nm][i][:], 0.0)
    b.memset(Sg[:], 0.0)
    b.memset(Sr[:], 0.0)
    b.memset(HS[1][:], 0.0)
    b.act(dv[:, 0:1], pcol(7), AF.Exp, scale=-1.0)
    b.act(dv[:, 0:1], dv[:, 0:1], AF.Ln, bias=1.0)
    b.ts(dv[:, 0:1], dv[:, 0:1], -8.0, None, ALU.mult)
    b.act(dv[:, 1:2], pcol(20), AF.Exp)
    b.ts(dv[:, 1:2], dv[:, 1:2], -1.0, None, ALU.mult)
    b.ts(dv[:, 2:3], pcol(32), -1.0, 1.0, ALU.mult, ALU.add)

    hTv = hT.rearrange("(k p) s -> p k s", p=128)

    def load_h(t):
        g.dma("sp", hTt[t % 2][:], hTv[:, :, t * T:(t + 1) * T], writes=[f"hTt{t % 2}"], chan=f"hTt{t % 2}")

    def proj_groups(t):
        par = t % 2
        fns = []
        for gi, (nm, c0, rows, H) in enumerate(GROUPS):
            def f(gi=gi, nm=nm, c0=c0, rows=rows, H=H):
                pj = PJ[gi % 2]
                for kk in range(16):
                    b.mm(pj[0:rows, 0:T], wcb[:, kk, c0:c0 + rows], hTt[par][:, kk, :], start=(kk == 0), stop=(kk == 15))
                dst = P[nm][par]
                b.cp(dst[0:rows, H:H + T], pj[0:rows, 0:T], eng="act")
                if H > 0 and t > 0:
                    b.cp(dst[0:rows, 0:H], P[nm][1 - par][0:rows, T:T + H], eng="pool")
            fns.append(f)
        return fns

    pending = []

    def fill(n):
        for _ in range(n):
            if pending:
                pending.pop(0)()

    def conv4(out, src, w0col, bias=None):
        if bias is None:
            b.ts(out, src[:, 0:T], pcol(w0col), None, ALU.mult)
        else:
            b.ts(out, src[:, 0:T], pcol(w0col), bias, ALU.mult, ALU.add)
        for j in range(1, 4):
            b.stt(out, src[:, j:j + T], pcol(w0col + j), out, ALU.mult, ALU.add)

    def inverse(N_, M_, R_, Pn, PTn, ncols, hs, psA, psB, psC):
        nb = ncols // 64
        nbk = (nb + 3) // 4
        banks = [psA, psC][:nbk] if nbk <= 2 else None
        PR = [PRa, PRb]

        def v4(tile_):
            return tile_[0:64, 0:nb * 128].rearrange("p (n two c) -> p n two c", two=2, c=64)

        def v3(tile_, ncol):
            return tile_[0:64, 0:ncol].rearrange("p (n c) -> p n c", c=64)
        for i in range(nb):
            sl = slice(i * 64, (i + 1) * 64)
            b.mm(psA[0:64, sl], M_[0:64, sl], N_[0:64, sl])
            b.mm(psB[0:64, sl], N_[0:64, sl], M_[0:64, sl])
        b.cp(v4(PR[0])[:, :, 0, :], v3(psA, ncols), eng="act")
        b.tt(v4(PR[0])[:, :, 1, :], v3(cm[:, IDR, :], ncols), v3(N_, ncols), ALU.subtract)
        b.cp(PTn[0:64, 0:ncols], psB[0:64, 0:ncols], eng="dve")
        fill(1)
        for k in range(1, 5):
            cur, nxt = PR[(k + 1) % 2], PR[k % 2]
            PTc, PTx = (PTn, M_) if k % 2 == 1 else (M_, PTn)
            for i in range(nb):
                sl = slice(i * 64, (i + 1) * 64)
                bank = (psA, psC)[i // 4]
                bi = i % 4
                b.mm(bank[0:64, bi * 128:(bi + 1) * 128], PTc[0:64, sl], cur[0:64, i * 128:(i + 1) * 128])
                b.mm(psB[0:64, sl], cur[0:64, i * 128:i * 128 + 64], PTc[0:64, sl])
            for h_ in range(nbk):
                nbh = min(4, nb - 4 * h_)
                bank = (psA, psC)[h_]
                bv = bank[0:64, 0:nbh * 128].rearrange("p (n two c) -> p n two c", two=2, c=64)
                nv_ = nxt[0:64, h_ * 512:h_ * 512 + nbh * 128].rearrange("p (n two c) -> p n two c", two=2, c=64)
                cv_ = cur[0:64, h_ * 512:h_ * 512 + nbh * 128].rearrange("p (n two c) -> p n two c", two=2, c=64)
                b.cp(nv_[:, :, 0, :], bv[:, :, 0, :], eng="act")
                b.tt(nv_[:, :, 1, :], cv_[:, :, 1, :], bv[:, :, 1, :], ALU.add)
            b.cp(PTx[0:64, 0:ncols], psB[0:64, 0:ncols], eng="dve")
            fill(1)
        cur = PR[0]
        PTc = PTn
        for i in range(nb):
            sl = slice(i * 64, (i + 1) * 64)
            b.mm(psB[0:64, sl], PTc[0:64, sl], cur[0:64, i * 128 + 64:(i + 1) * 128])
        b.tt(v3(R_, ncols), v4(cur)[:, :, 1, :], v3(psB, ncols), ALU.add)
        fill(1)

    def lru(t):
        par = t % 2
        lx = P["lx"][par]
        lg = P["lg"][par]
        xc, r_, i_, m_ = A[0], A[1], A[2], A[3]
        conv4(xc[:], lx, 0, bias=pcol(4))
        b.mm(M[0][:, 0:T], mats[:, 0, :], xc[:])
        b.mm(M[1][:, 0:T], mats[:, 1, :], xc[:])
        b.act(r_[:], M[0][:, 0:T], AF.Sigmoid, bias=pcol(5))
        b.act(i_[:], M[1][:, 0:T], AF.Sigmoid, bias=pcol(6))
        b.act(r_[:], r_[:], AF.Exp, scale=dv[:, 0:1])
        b.act(m_[:], r_[:], AF.Square)
        b.act(m_[:], m_[:], AF.Sqrt, bias=1.0, scale=-1.0)
        b.tt(i_[:], i_[:], xc[:], ALU.mult)
        b.tt(i_[:], i_[:], m_[:], ALU.mult)
        b.scan(HS[par][:], r_[:], i_[:], HS[1 - par][:, T - 1:T])
        gsc = A[0]
        b.act(gsc[:], lg[:, 0:T], AF.Square)
        b.ts(gsc[:], gsc[:], 0.044715, 1.0, ALU.mult, ALU.add)
        b.tt(gsc[:], gsc[:], lg[:, 0:T], ALU.mult)
        b.act(gsc[:], gsc[:], AF.Sigmoid, scale=1.5957691216057308)
        b.tt(gsc[:], gsc[:], lg[:, 0:T], ALU.mult)
        b.tt(YO[0][par][:], gsc[:], HS[par][:], ALU.mult)
        g.dma("sp", yd[0, :, t * T:(t + 1) * T], YO[0][par][:], reads=[f"Y0{par}"], chan=f"yo0{par}")

    def gdn(t):
        par = t % 2
        q_, k_, v_ = A[4], A[5], A[6]
        tmp, beta_b, gb, gc_b, egc, kd, Kbeta, Kbe, qd = A[7], A[8], A[9], A[10], A[11], A[12], A[13], A[14], A[15]
        conv4(q_[:], P["q"][par], 8)
        conv4(k_[:], P["k"][par], 12)
        conv4(v_[:], P["v"][par], 16)
        for x_ in (q_, k_, v_):
            b.act(x_[:], x_[:], AF.Silu)
        for x_, sc in ((q_, 128.0 ** -0.5), (k_, 1.0)):
            b.act(tmp[:], x_[:], AF.Square)
            b.mm(M[0][:, 0:T], ones, tmp[:])
            b.act(tmp[:], M[0][:, 0:T], AF.Sqrt, bias=EPS)
            b.recip(tmp[:], tmp[:])
            b.stt(x_[:], x_[:], sc, tmp[:], ALU.mult, ALU.mult)
        ba = P["ba"][par]
        b.mm(M[0][:, 0:T], mats[0:2, 8, :], ba[0:2, 0:T])
        b.mm(M[1][:, 0:T], mats[0:2, 9, :], ba[0:2, 0:T])
        b.act(beta_b[:], M[0][:, 0:T], AF.Sigmoid)
        b.act(gb[:], M[1][:, 0:T], AF.Exp, bias=pcol(21))
        b.act(gb[:], gb[:], AF.Ln, bias=1.0)
        b.ts(gb[:], gb[:], dv[:, 1:2], None, ALU.mult)
        b.scan(gc_b[:], cm[:, RST, 0:T], gb[:], 0.0)
        for n in range(NCH):
            sl = slice(n * C, (n + 1) * C)
            b.ts(kd[:, sl], gc_b[:, sl], gc_b[:, n * C + C - 1:n * C + C], None, ALU.subtract)
        b.act(kd[:], kd[:], AF.Exp, scale=-1.0)
        b.act(egc[:], gc_b[:], AF.Exp)
        b.tt(kd[:], kd[:], k_[:], ALU.mult)
        b.tt(Kbeta[:], k_[:], beta_b[:], ALU.mult)
        b.tt(Kbe[:], Kbeta[:], egc[:], ALU.mult)
        b.tt(v_[:], v_[:], beta_b[:], ALU.mult)
        b.tt(qd[:], q_[:], egc[:], ALU.mult)
        for n in range(NCH):
            b.tr(M[2][0:64, n * 32:(n + 1) * 32], gc_b[0:32, n * C:(n + 1) * C], ident[0:32, 0:32])
        b.cp(gcolT[:, 0:NCH], M[2][0:64, 0:NCH * 32:32])
        DT_, D_, DTsu, R_, Pn, PTn = Bt[0], Bt[1], Bt[2], Bt[3], Bt[4], Bt[5]
        for n in range(NCH):
            sl = slice(n * C, (n + 1) * C)
            b.ts(DT_[0:64, sl], gc_b[0:64, sl], gcolT[:, n:n + 1], 0.0, ALU.subtract, ALU.min)
            b.ts(D_[0:64, sl], gc_b[0:64, sl], gcolT[:, n:n + 1], 0.0, ALU.subtract, ALU.max)
        b.act(DT_[0:64, 0:T], DT_[0:64, 0:T], AF.Exp)
        b.act(D_[0:64, 0:T], D_[0:64, 0:T], AF.Exp, scale=-1.0)
        b.tt(DTsu[0:64, 0:T], DT_[0:64, 0:T], cm[0:64, MSU, 0:T], ALU.mult)
        b.tt(D_[0:64, 0:T], D_[0:64, 0:T], cm[0:64, MSL, 0:T], ALU.mult)
        b.tt(DT_[0:64, 0:T], DT_[0:64, 0:T], cm[0:64, MUI, 0:T], ALU.mult)
        for n in range(NCH):
            sl = slice(n * C, (n + 1) * C)
            b.mm(M[0][0:64, sl], k_[:, sl], Kbeta[:, sl])
            b.mm(M[1][0:64, sl], Kbeta[:, sl], k_[:, sl])
            b.mm(M[2][0:64, sl], k_[:, sl], q_[:, sl])
        b.tt(DTsu[0:64, 0:T], DTsu[0:64, 0:T], M[0][0:64, 0:T], ALU.mult)
        b.tt(D_[0:64, 0:T], D_[0:64, 0:T], M[1][0:64, 0:T], ALU.mult)
        b.tt(DT_[0:64, 0:T], DT_[0:64, 0:T], M[2][0:64, 0:T], ALU.mult)
        inverse(DTsu, D_, R_, Pn, PTn, T, None, M[0], M[1], M[2])
        Kbe_tm, Vb_tm, Kd_tm, U0, WkT = Bt[6], Bt[7], Bt[8], Bt[9], A[13]
        for n in range(NCH):
            sl = slice(n * C, (n + 1) * C)
            b.tr(M[3][0:64, n * 128:(n + 1) * 128], Kbe[:, sl], ident)
            b.tr(M[4][0:64, n * 128:(n + 1) * 128], v_[:, sl], ident)
            b.tr(M[5][0:64, n * 128:(n + 1) * 128], kd[:, sl], ident)
        b.cp(Kbe_tm[0:64, :], M[3][0:64, :], eng="act")
        b.cp(Vb_tm[0:64, :], M[4][0:64, :], eng="dve")
        b.cp(Kd_tm[0:64, :], M[5][0:64, :], eng="act")
        for n in range(NCH):
            sl = slice(n * C, (n + 1) * C)
            b.mm(M[0][0:64, n * 128:(n + 1) * 128], R_[0:64, sl], Vb_tm[0:64, n * 128:(n + 1) * 128])
            b.mm(M[1][:, sl], Kbe_tm[0:64, n * 128:(n + 1) * 128], R_[0:64, sl])
        b.cp(U0[0:64, :], M[0][0:64, :], eng="dve")
        b.cp(WkT[:], M[1][:, 0:T], eng="act")
        for n in range(NCH):
            sl = slice(n * C, (n + 1) * C)
            b.mm(M[2][0:64, 0:128], WkT[:, sl], Sg[:, :])
            b.tt(Usb[:, :], U0[0:64, n * 128:(n + 1) * 128], M[2][0:64, 0:128], ALU.subtract)
            b.mm(M[3][:, sl], Sg[:, :], qd[:, sl], start=True, stop=False)
            b.mm(M[3][:, sl], Usb[:, :], DT_[0:64, sl], start=False, stop=True)
            b.mm(M[4][:, 0:128], Kd_tm[0:64, n * 128:(n + 1) * 128], Usb[:, :])
            b.stt(Sg[:, :], Sg[:, :], egc[:, n * C + C - 1:n * C + C], M[4][:, 0:128], ALU.mult, ALU.add)
            fill(1)
        osb, sz = A[7], A[8]
        b.act(tmp[:], M[3][:, 0:T], AF.Square)
        b.mm(M[0][:, 0:T], ones, tmp[:])
        b.act(tmp[:], M[0][:, 0:T], AF.Sqrt, bias=EPS, scale=1.0 / 128)
        b.recip(tmp[:], tmp[:])
        b.tt(tmp[:], tmp[:], M[3][:, 0:T], ALU.mult)
        b.act(sz[:], P["z"][par][:, 0:T], AF.Silu)
        b.stt(YO[1][par][:], tmp[:], pcol(22), sz[:], ALU.mult, ALU.mult)
        g.dma("sp", yd[1, :, t * T:(t + 1) * T], YO[1][par][:], reads=[f"Y1{par}"], chan=f"yo1{par}")

    def rwkv(t):
        par = t % 2
        r_, k_, v_, wa_, g1_, g2_ = A[0], A[1], A[2], A[3], A[4], A[5]
        tmp = A[6]
        for dst, nm, mucol, rows in ((r_, "r", 23, 128), (k_, "rk", 24, 128), (v_, "rv", 25, 128), (wa_, "wa", 26, 128),
                                     (g1_, "g1", 27, 128), (g2_, "g2", 28, 32)):
            src = P[nm][par]
            b.tt(tmp[0:rows, :], src[0:rows, 0:T], src[0:rows, 1:T + 1], ALU.subtract)
            b.stt(dst[0:rows, :], tmp[0:rows, :], pv[0:rows, mucol:mucol + 1], src[0:rows, 1:T + 1], ALU.mult, ALU.add)
        ld, a_, g_, kk, p_, cum = A[7], A[8], A[9], A[10], A[11], A[12]
        b.act(tmp[0:64, :], wa_[0:64, :], AF.Tanh)
        b.mm(M[0][:, 0:T], mats[0:64, 2, :], tmp[0:64, :])
        b.act(ld[:], M[0][:, 0:T], AF.Sigmoid, bias=pcol(29))
        b.ts(ld[:], ld[:], -0.6065306597126334, None, ALU.mult)
        b.mm(M[1][:, 0:T], mats[64:128, 2, :], wa_[64:128, :])
        b.act(a_[:], M[1][:, 0:T], AF.Sigmoid, bias=pcol(30))
        b.act(g1_[:], g1_[:], AF.Sigmoid)
        b.act(g2_[0:32, :], g2_[0:32, :], AF.Sigmoid)
        b.mm(M[2][:, 0:T], mats[:, 3, :], g1_[:], start=True, stop=False)
        b.mm(M[2][:, 0:T], mats[0:32, 4, :], g2_[0:32, :], start=False, stop=True)
        b.cp(g_[:], M[2][:, 0:T], eng="act")
        b.ts(kk[:], k_[:], pcol(31), None, ALU.mult)
        b.act(tmp[:], kk[:], AF.Square)
        b.mm(M[0][:, 0:T], blk64, tmp[:])
        b.act(tmp[:], M[0][:, 0:T], AF.Sqrt, bias=EPS)
        b.recip(tmp[:], tmp[:])
        b.tt(kk[:], kk[:], tmp[:], ALU.mult)
        b.ts(tmp[:], a_[:], pcol(32), dv[:, 2:3], ALU.mult, ALU.add)
        b.tt(k_[:], k_[:], tmp[:], ALU.mult)
        b.tt(p_[:], kk[:], a_[:], ALU.mult)
        rkb = A[13]
        b.stt(rkb[:], r_[:], pcol(33), k_[:], ALU.mult, ALU.mult)
        b.scan(cum[:], cm[:, RST, 0:T], ld[:], 0.0)
        ecum, encum, ecx, ecl = A[14], A[15], A[16], A[17]
        b.act(ecum[:], cum[:], AF.Exp)
        b.act(encum[:], cum[:], AF.Exp, scale=-1.0)
        b.tt(ecx[:], cum[:], ld[:], ALU.subtract)
        b.act(ecx[:], ecx[:], AF.Exp)
        for n in range(NCH):
            sl = slice(n * C, (n + 1) * C)
            b.ts(ecl[:, sl], cum[:, sl], cum[:, n * C + C - 1:n * C + C], None, ALU.subtract)
        b.act(ecl[:], ecl[:], AF.Exp, scale=-1.0)
        Rh, Kt, Pt, KKh, Kbar, Pbar = A[18], A[19], A[20], A[21], A[6], A[7]
        b.tt(Rh[:], r_[:], ecum[:], ALU.mult)
        b.tt(Kt[:], k_[:], encum[:], ALU.mult)
        b.tt(Pt[:], p_[:], encum[:], ALU.mult)
        b.tt(KKh[:], kk[:], ecx[:], ALU.mult)
        b.tt(Kbar[:], k_[:], ecl[:], ALU.mult)
        b.tt(Pbar[:], p_[:], ecl[:], ALU.mult)
        N_, M_, AkvT, ArkT, nArpT, R_, Pn, PTn = Bt[0], Bt[1], Bt[2], Bt[3], Bt[4], Bt[5], Bt[10], Bt[11]

        def blk(h, n):
            return slice((h * NCH + n) * 64, (h * NCH + n + 1) * 64)

        specs = [(N_, Pt, KKh, MSU), (M_, KKh, Pt, MSL), (AkvT, Kt, KKh, MSU), (ArkT, Kt, Rh, MUI), (nArpT, Pt, Rh, NMUI)]
        for si, (dst, lt, rt, msk) in enumerate(specs):
            for h in range(2):
                hp = slice(64 * h, 64 * h + 64)
                psb = M[(2 * si + h) % 6]
                for n in range(NCH):
                    sl = slice(n * C, (n + 1) * C)
                    b.mm(psb[0:64, sl], lt[hp, sl], rt[hp, sl])
                b.tt(dst[0:64, h * T:(h + 1) * T], psb[0:64, 0:T], cm[0:64, msk, 0:T], ALU.mult)
        inverse(N_, M_, R_, Pn, PTn, 512, None, M[0], M[1], M[2])
        V_tm, KK_tm, Kb_tm, nPb_tm = Bt[6], Bt[7], Bt[8], Bt[9]
        for n in range(NCH):
            sl = slice(n * C, (n + 1) * C)
            b.tr(M[0][0:64, n * 128:(n + 1) * 128], v_[:, sl], ident)
            b.tr(M[1][0:64, n * 128:(n + 1) * 128], KKh[:, sl], ident)
            b.tr(M[2][0:64, n * 128:(n + 1) * 128], Kbar[:, sl], ident)
            b.tr(M[3][0:64, n * 128:(n + 1) * 128], Pbar[:, sl], ident)
        b.cp(V_tm[0:64, :], M[0][0:64, :], eng="act")
        b.cp(KK_tm[0:64, :], M[1][0:64, :], eng="dve")
        b.cp(Kb_tm[0:64, :], M[2][0:64, :], eng="act")
        b.ts(nPb_tm[0:64, :], M[3][0:64, :], -1.0, None, ALU.mult)
        Y_, U0, W1T = Bt[12], Bt[13], A[8]

        def tm(n, h):
            return slice(n * 128 + h * 64, n * 128 + h * 64 + 64)

        for n in range(NCH):
            for h in range(2):
                b.mm(M[4][0:64, tm(n, h)], AkvT[0:64, blk(h, n)], V_tm[0:64, tm(n, h)])
        b.cp(Y_[0:64, :], M[4][0:64, :], eng="dve")
        for n in range(NCH):
            for h in range(2):
                b.mm(M[5][0:64, tm(n, h)], R_[0:64, blk(h, n)], Y_[0:64, tm(n, h)])
                b.mm(M[0][64 * h:64 * h + 64, n * C:(n + 1) * C], KK_tm[0:64, tm(n, h)], R_[0:64, blk(h, n)])
        b.cp(U0[0:64, :], M[5][0:64, :], eng="dve")
        b.cp(W1T[:], M[0][:, 0:T], eng="act")
        for n in range(NCH):
            sl = slice(n * C, (n + 1) * C)
            b.mm(M[1][0:64, 0:128], W1T[:, sl], Sr[:, :])
            b.tt(Usb[:, :], U0[0:64, n * 128:(n + 1) * 128], M[1][0:64, 0:128], ALU.add)
            b.mm(M[2][:, sl], Sr[:, :], Rh[:, sl], start=True, stop=False)
            for h in range(2):
                hp = slice(64 * h, 64 * h + 64)
                last = (h == 1)
                b.mm(M[2][hp, sl], V_tm[0:64, tm(n, h)], ArkT[0:64, blk(h, n)], start=False, stop=False)
                b.mm(M[2][hp, sl], Usb[:, 64 * h:64 * h + 64], nArpT[0:64, blk(h, n)], start=False, stop=True)
                b.mm(M[3][hp, 64 * h:64 * h + 64], Kb_tm[0:64, tm(n, h)], V_tm[0:64, tm(n, h)], start=True, stop=False)
                b.mm(M[3][hp, 64 * h:64 * h + 64], nPb_tm[0:64, tm(n, h)], Usb[:, 64 * h:64 * h + 64], start=False, stop=True)
            for h in range(2):
                hp = slice(64 * h, 64 * h + 64)
                hc = slice(64 * h, 64 * h + 64)
                b.stt(Sr[hp, hc], Sr[hp, hc], ecum[hp, n * C + C - 1:n * C + C], M[3][hp, hc], ALU.mult, ALU.add)
            fill(1)
        osb, cen = A[9 + 1], A[11]
        b.cp(osb[:], M[2][:, 0:T], eng="act")
        b.mm(M[4][:, 0:T], blk64, osb[:])
        b.stt(cen[:], M[4][:, 0:T], -1.0 / 64, osb[:], ALU.mult, ALU.add)
        b.act(tmp[:], cen[:], AF.Square)
        b.mm(M[5][:, 0:T], blk64, tmp[:])
        b.act(tmp[:], M[5][:, 0:T], AF.Sqrt, bias=64e-5, scale=1.0 / 64)
        b.recip(tmp[:], tmp[:])
        b.tt(cen[:], cen[:], tmp[:], ALU.mult)
        b.ts(cen[:], cen[:], pcol(34), pcol(35), ALU.mult, ALU.add)
        b.mm(M[4][:, 0:T], blk64, rkb[:])
        b.tt(tmp[:], M[4][:, 0:T], v_[:], ALU.mult)
        b.tt(cen[:], cen[:], tmp[:], ALU.add)
        b.tt(YO[2][par][:], cen[:], g_[:], ALU.mult)
        g.dma("sp", yd[2, :, t * T:(t + 1) * T], YO[2][par][:], reads=[f"Y2{par}"], chan=f"yo2{par}")

    branches = "abc"
    load_h(0)
    for f in proj_groups(0):
        f()
    for t in range(NT):
        if t + 1 < NT:
            load_h(t + 1)
            pending.extend(proj_groups(t + 1))
        if "a" in branches:
            lru(t)
        if "b" in branches:
            gdn(t)
        if "c" in branches:
            rwkv(t)
        fill(len(pending))
    g.emit()
    return nc


def la_cols(c):
    cols = []
    for base in (0, 1024, 2048, 3072, 4096, 5120, 6160, 7184, 8208):
        cols += list(range(base + c * 128, base + c * 128 + 128))
    cols += list(range(9232, 9360))
    cols += list(range(9360, 9520))
    cols += [6144 + c, 6152 + c]
    return np.array(cols)


def la_consts():
    cmv = np.zeros((128, NCM, 512), np.float32)
    j = np.arange(64)[:, None]
    i = np.arange(512)[None, :] % 64
    cmv[:, 0, :] = 1.0
    cmv[:, 0, ::64] = 0.0
    cmv[0:64, 1, :] = (j < i)
    cmv[0:64, 2, :] = (j > i)
    cmv[0:64, 3, :] = (j <= i)
    cmv[0:64, 4, :] = -1.0 * (j <= i)
    cmv[0:64, 5, :] = (j == i)
    return cmv


def la_inputs(inp, l, c):
    f = np.float32
    sl = slice(c * 128, (c + 1) * 128)
    pv = np.zeros((128, NPV), f)
    pv[:, 0:4] = inp["lru_conv_w"][l][:, sl].T
    pv[:, 4] = inp["lru_conv_b"][l][sl]
    pv[:, 5] = inp["lru_b_a"][l][sl]
    pv[:, 6] = inp["lru_b_x"][l][sl]
    pv[:, 7] = inp["lru_lambda"][l][sl]
    gw = inp["gdn_conv_w"][l]
    pv[:, 8:12] = gw[:, c * 128:(c + 1) * 128].T
    pv[:, 12:16] = gw[:, 1024 + c * 128:1024 + (c + 1) * 128].T
    pv[:, 16:20] = gw[:, 2048 + c * 128:2048 + (c + 1) * 128].T
    pv[:, 20] = inp["gdn_a_log"][l][c]
    pv[:, 21] = inp["gdn_dt_bias"][l][c]
    pv[:, 22] = inp["gdn_norm_w"][l]
    mu = inp["rwkv_mu"][l]
    pv[:, 23] = mu[c * 128:(c + 1) * 128]
    pv[:, 24] = mu[1024 + c * 128:1024 + (c + 1) * 128]
    pv[:, 25] = mu[2048 + c * 128:2048 + (c + 1) * 128]
    pv[:, 26] = mu[3072:3200]
    pv[:, 27] = mu[3200:3328]
    pv[0:32, 28] = mu[3328:3360]
    pv[:, 29] = inp["rwkv_w0"][l][sl]
    pv[:, 30] = inp["rwkv_a0"][l][sl]
    pv[:, 31] = inp["rwkv_k_k"][l][sl]
    pv[:, 32] = inp["rwkv_k_a"][l][sl]
    pv[:, 33] = inp["rwkv_r_k"][l].reshape(-1)[sl]
    pv[:, 34] = inp["rwkv_ln_w"][l][sl]
    pv[:, 35] = inp["rwkv_ln_b"][l][sl]
    mats = np.zeros((128, NM, 128), f)
    for gl in range(2):
        mats[gl * 64:(gl + 1) * 64, 0, gl * 64:(gl + 1) * 64] = inp["lru_w_a"][l][2 * c + gl]
        mats[gl * 64:(gl + 1) * 64, 1, gl * 64:(gl + 1) * 64] = inp["lru_w_x"][l][2 * c + gl]
    mats[0:64, 2, :] = inp["rwkv_w_up"][l][:, sl]
    mats[64:128, 2, :] = inp["rwkv_a_up"][l][:, sl]
    mats[:, 3, :] = inp["rwkv_g_up"][l][0:128, sl]
    mats[0:32, 4, :] = inp["rwkv_g_up"][l][128:160, sl]
    mats[:, 5, :] = np.eye(128, dtype=f)
    mats[:, 6, :] = 1.0
    mats[0:64, 7, 0:64] = 1.0
    mats[64:128, 7, 64:128] = 1.0
    mats[0, 8, :] = 1.0
    mats[1, 9, :] = 1.0
    wc = np.ascontiguousarray(inp["w_in"][l][:, la_cols(c)])
    return dict(wc=wc, pvd=pv, matsd=mats, cmd=la_consts())


TB = 512
DFF = 5632
NF = DFF // 128


def build_LB(NTOK, mode="full"):
    NTL = NTOK // TB
    nc = bass.Bass("TRN2", target_bir_lowering=False)
    xTd = nc.dram_tensor("xT", [D, NTOK], F32, kind="ExternalInput").ap()
    nvd = nc.dram_tensor("nvd", [128, 16, 4], F32, kind="ExternalInput").ap()
    onesd = nc.dram_tensor("onesd", [128, 128], F32, kind="ExternalInput").ap()
    hTo = nc.dram_tensor("hT_out", [D, NTOK], BF16, kind="ExternalOutput").ap()
    full = (mode == "full")
    if full:
        yTd = nc.dram_tensor("yT", [3072, NTOK], BF16, kind="ExternalInput").ap()
        hTd = nc.dram_tensor("hT", [D, NTOK], BF16, kind="ExternalInput").ap()
        wg3 = nc.dram_tensor("wg3", [D, 6144], F32, kind="ExternalInput").ap()
        wbr = nc.dram_tensor("wbr", [3072, D], F32, kind="ExternalInput").ap()
        wo = nc.dram_tensor("wo", [D, D], F32, kind="ExternalInput").ap()
        wfg = nc.dram_tensor("wfg", [D, DFF], F32, kind="ExternalInput").ap()
        wfu = nc.dram_tensor("wfu", [D, DFF], F32, kind="ExternalInput").ap()
        wfd = nc.dram_tensor("wfd", [DFF, D], F32, kind="ExternalInput").ap()
        xTo = nc.dram_tensor("xT_out", [D, NTOK], F32, kind="ExternalOutput").ap()
    g = G(nc)
    b = Bld(g)
    nv = g.sb([128, 16, 4], F32, "nv")
    ones = g.sb([128, 128], F32, "ones")
    xt = g.sb([128, 16, TB], F32, "xt")
    hb = g.sb([128, 16, TB], BF16, "hb")
    rstd = g.sb([128, TB], F32, "rstd")
    sq = [g.sb([128, TB], F32, f"sq{i}") for i in range(2)]
    SS = g.ps([128, 512], F32, "SS")
    g.dma("sp", nv[:], nvd, writes=["nv"], chan="nv")
    g.dma("sp", ones[:], onesd, writes=["ones"], chan="ones")
    if full:
        big = g.sb([128, NF * TB], BF16, "big")
        yt = big[:, 0:24 * TB].rearrange("p (k t) -> p k t", t=TB)
        mixT = big[:, 24 * TB:40 * TB].rearrange("p (k t) -> p k t", t=TB)
        aT = big[:, :].rearrange("p (k t) -> p k t", t=TB)
        oT = g.sb([128, 16, TB], F32, "oT")
        WP = [g.sb([128, 12288], BF16, f"WP{i}") for i in range(2)]
        gsb = [g.sb([128, TB], F32, f"gsb{i}") for i in range(2)]
        tmpb = [g.sb([128, TB], F32, f"tmpb{i}") for i in range(2)]
        macc = g.sb([128, 4, TB], F32, "macc")
        pA = [g.ps([128, 512], F32, f"pA{i}") for i in range(2)]
        pB = [g.ps([128, 512], F32, f"pB{i}") for i in range(2)]

    xTv = xTd.rearrange("(k p) s -> p k s", p=128)
    hTov = hTo.rearrange("(k p) s -> p k s", p=128)

    def norm_stats_finish():
        b.act(rstd[:], SS[:, 0:TB], AF.Sqrt, bias=EPS, scale=1.0 / D)
        b.recip(rstd[:], rstd[:])

    def sumsq(src_fn):
        for k in range(16):
            s_ = sq[k % 2]
            b.act(s_[:], src_fn(k), AF.Square)
            b.mm(SS[:, 0:TB], ones[:, :], s_[:], start=(k == 0), stop=(k == 15))
        norm_stats_finish()

    def residual_add(j):
        for k in range(16):
            t_ = sq[k % 2]
            b.stt(t_[:], oT[:, k, :], nv[:, k, j:j + 1], rstd[:], ALU.mult, ALU.mult)
            b.tt(xt[:, k, :], xt[:, k, :], t_[:], ALU.add)

    def norm_to(dst, j):
        sumsq(lambda k: xt[:, k, :])
        for k in range(16):
            b.stt(dst[:, k, :], xt[:, k, :], nv[:, k, j:j + 1], rstd[:], ALU.mult, ALU.mult)

    items = []

    def wview(buf, off, K, n):
        return buf[:, off:off + K * n].rearrange("p (k n) -> p k n", n=n)

    for tl in range(NTL):
        tsl = slice(tl * TB, (tl + 1) * TB)

        def start_tile(buf, tsl=tsl):
            g.dma("sp", xt[:], xTv[:, :, tsl], writes=["xt"], chan="xt")
            if full:
                g.dma("sp", hb[:], hTd.rearrange("(k p) s -> p k s", p=128)[:, :, tsl], writes=["hb"], chan="hb")
                g.dma("sp", yt, yTd.rearrange("(k p) s -> p k s", p=128)[:, :, tsl], writes=["big"], chan="big")

        if not full:
            def only_norm(buf, tsl=tsl, st=start_tile):
                st(None)
                norm_to(hb, 3)
                g.dma("sp", hTov[:, :, tsl], hb[:], reads=["hb"], chan="hbo")
            items.append((None, only_norm))
            continue

        for dcg in range(4):
            for br in range(3):
                def ld(buf, dcg=dcg, br=br):
                    g.dma("pool", wview(buf, 0, 16, 512), wg3.rearrange("(k p) n -> p k n", p=128)[:, :, br * 2048 + dcg * 512: br * 2048 + dcg * 512 + 512],
                          writes=[buf.name], chan=buf.name)
                    g.dma("pool", wview(buf, 8192, 8, 512), wbr.rearrange("(k p) n -> p k n", p=128)[:, br * 8:br * 8 + 8, dcg * 512:dcg * 512 + 512],
                          writes=[buf.name], chan=buf.name)

                def cmp(buf, dcg=dcg, br=br, first=(dcg == 0 and br == 0), tsl=tsl, st=start_tile):
                    if first:
                        st(None)
                    Wg = wview(buf, 0, 16, 512)
                    Wb = wview(buf, 8192, 8, 512)
                    for dci in range(4):
                        dc = dcg * 4 + dci
                        cs = slice(dci * 128, dci * 128 + 128)
                        pg, pb = pA[dci % 2], pB[dci % 2]
                        for k in range(16):
                            b.mm(pg[:, 0:TB], Wg[:, k, cs], hb[:, k, :], start=(k == 0), stop=(k == 15))
                        b.act(gsb[dci % 2][:], pg[:, 0:TB], AF.Sigmoid)
                        for k in range(8):
                            b.mm(pb[:, 0:TB], Wb[:, k, cs], yt[:, br * 8 + k, :], start=(k == 0), stop=(k == 7))
                        if br == 0:
                            b.tt(macc[:, dci, :], gsb[dci % 2][:], pb[:, 0:TB], ALU.mult)
                        elif br == 1:
                            b.tt(tmpb[dci % 2][:], gsb[dci % 2][:], pb[:, 0:TB], ALU.mult)
                            b.tt(macc[:, dci, :], macc[:, dci, :], tmpb[dci % 2][:], ALU.add)
                        else:
                            b.tt(tmpb[dci % 2][:], gsb[dci % 2][:], pb[:, 0:TB], ALU.mult)
                            b.tt(mixT[:, dc, :], macc[:, dci, :], tmpb[dci % 2][:], ALU.add)
                items.append((ld, cmp))

        def dense_items(w_ap, K, ncols_total, pcols, rhs_fn, evac_fn, post_fn=None):
            npan = ncols_total // pcols
            for pi in range(npan):
                def ld(buf, pi=pi):
                    g.dma("pool", wview(buf, 0, K, pcols), w_ap.rearrange("(k p) n -> p k n", p=128)[:, :, pi * pcols:(pi + 1) * pcols],
                          writes=[buf.name], chan=buf.name)

                def cmp(buf, pi=pi, lastp=(pi == npan - 1)):
                    W = wview(buf, 0, K, pcols)
                    for ci in range(pcols // 128):
                        dc = pi * (pcols // 128) + ci
                        ps_ = pA[dc % 2]
                        for k in range(K):
                            b.mm(ps_[:, 0:TB], W[:, k, ci * 128:(ci + 1) * 128], rhs_fn(k), start=(k == 0), stop=(k == K - 1))
                        evac_fn(dc, ps_)
                    if lastp and post_fn is not None:
                        post_fn()
                items.append((ld, cmp))

        def evac_o(dc, ps_):
            b.cp(oT[:, dc, :], ps_[:, 0:TB], eng="act")
            s_ = sq[dc % 2]
            b.act(s_[:], ps_[:, 0:TB], AF.Square)
            b.mm(SS[:, 0:TB], ones[:, :], s_[:], start=(dc == 0), stop=(dc == 15))

        def post_mix():
            norm_stats_finish()
            residual_add(0)
            norm_to(hb, 1)

        def post_ffn(tsl=tsl):
            norm_stats_finish()
            residual_add(2)
            g.dma("sp", xTo.rearrange("(k p) s -> p k s", p=128)[:, :, tsl], xt[:], reads=["xt"], chan="xto")
            norm_to(hb, 3)
            g.dma("sp", hTov[:, :, tsl], hb[:], reads=["hb"], chan="hbo")

        dense_items(wo, 16, D, 512, lambda k: mixT[:, k, :], evac_o, post_mix)
        for fi in range(DFF // 256):
            def ld(buf, fi=fi):
                g.dma("pool", wview(buf, 0, 16, 256), wfg.rearrange("(k p) n -> p k n", p=128)[:, :, fi * 256:fi * 256 + 256], writes=[buf.name], chan=buf.name)
                g.dma("pool", wview(buf, 4096, 16, 256), wfu.rearrange("(k p) n -> p k n", p=128)[:, :, fi * 256:fi * 256 + 256], writes=[buf.name], chan=buf.name)

            def cmp(buf, fi=fi):
                Wg_ = wview(buf, 0, 16, 256)
                Wu_ = wview(buf, 4096, 16, 256)
                for ci in range(2):
                    fc = fi * 2 + ci
                    pg, pu = pA[ci], pB[ci]
                    for k in range(16):
                        b.mm(pg[:, 0:TB], Wg_[:, k, ci * 128:(ci + 1) * 128], hb[:, k, :], start=(k == 0), stop=(k == 15))
                    for k in range(16):
                        b.mm(pu[:, 0:TB], Wu_[:, k, ci * 128:(ci + 1) * 128], hb[:, k, :], start=(k == 0), stop=(k == 15))
                    b.act(gsb[ci][:], pg[:, 0:TB], AF.Silu)
                    b.tt(aT[:, fc, :], gsb[ci][:], pu[:, 0:TB], ALU.mult)
            items.append((ld, cmp))
        dense_items(wfd, NF, D, 256, lambda k: aT[:, k, :], evac_o, post_ffn)

    nb = 0
    bufs = []
    for it in items:
        bufs.append(None)
    def buf_of(i):
        return WP[i % 2] if full else None
    if items and items[0][0] is not None:
        items[0][0](buf_of(0))
    for i, (ld, cmp) in enumerate(items):
        if i + 1 < len(items) and items[i + 1][0] is not None:
            items[i + 1][0](buf_of(i + 1))
        cmp(buf_of(i))
    g.emit()
    return nc


def lb_nv(inp, l):
    nv = np.zeros((128, 16, 4), np.float32)
    vecs = [inp["norm_mix_post"][l], inp["norm_ffn_pre"][l], inp["norm_ffn_post"][l],
            inp["norm_mix_pre"][l + 1] if l + 1 < inp["norm_mix_pre"].shape[0] else np.ones(D, np.float32)]
    for j, v in enumerate(vecs):
        nv[:, :, j] = v.reshape(16, 128).T
    return nv


def _run(nc, maps):
    return run_bass_kernel_spmd(nc, maps, core_ids=list(range(len(maps)))).results


def kernel(**inputs):
    inp = {k: np.asarray(v) for k, v in inputs.items()}
    f32 = np.float32
    NCORE = 8
    x = inp["x"][0].astype(f32)
    S = x.shape[0]
    NTOK = S // NCORE
    L = inp["w_in"].shape[0]
    ones = np.ones((128, 128), f32)
    xT_sh = [np.ascontiguousarray(x[c * NTOK:(c + 1) * NTOK].T) for c in range(NCORE)]
    nv0 = np.zeros((128, 16, 4), f32)
    nv0[:, :, 3] = inp["norm_mix_pre"][0].reshape(16, 128).T
    nc0 = build_LB(NTOK, "norm")
    r0 = _run(nc0, [dict(xT=xT_sh[c], nvd=nv0, onesd=ones) for c in range(NCORE)])
    hT_sh = [r0[c]["hT_out"] for c in range(NCORE)]
    ncA = build_LA(S)
    ncB = build_LB(NTOK, "full")
    for l in range(L):
        hT_all = np.ascontiguousarray(np.concatenate(hT_sh, axis=1))
        mapsA = []
        for c in range(NCORE):
            m = la_inputs(inp, l, c)
            m["hT"] = hT_all
            mapsA.append(m)
        rA = _run(ncA, mapsA)
        yfull = np.stack([rA[c]["y"] for c in range(NCORE)], axis=1).reshape(3072, S)
        del rA, mapsA
        wg3 = np.ascontiguousarray(inp["w_in"][l][:, 9520:])
        wbr = np.ascontiguousarray(inp["w_branch"][l].reshape(3072, D))
        nvl = lb_nv(inp, l)
        mapsB = [dict(xT=xT_sh[c], yT=np.ascontiguousarray(yfull[:, c * NTOK:(c + 1) * NTOK]), hT=hT_sh[c],
                      nvd=nvl, onesd=ones, wg3=wg3, wbr=wbr, wo=inp["w_out"][l], wfg=inp["ffn_w_gate"][l],
                      wfu=inp["ffn_w_up"][l], wfd=inp["ffn_w_down"][l]) for c in range(NCORE)]
        rB = _run(ncB, mapsB)
        xT_sh = [rB[c]["xT_out"] for c in range(NCORE)]
        hT_sh = [rB[c]["hT_out"] for c in range(NCORE)]
        del rB, mapsB
    out = np.concatenate([np.asarray(xT_sh[c], dtype=f32).T for c in range(NCORE)], axis=0)
    return np.ascontiguousarray(out[None]).astype(f32)
```
